# Optimizing a Trainium2 kernel written in Bass

```python
import math
import jax, jax.numpy as jnp
from jax import lax
import numpy as np

D_MODEL = 1024
BATCH = 8
SEQ = 4096
DEPTH = 1

PLE_DIM = 256
EPS = 1e-6
SSD_HEADS = 16
SSD_HEAD_DIM = 64
SSD_INNER = SSD_HEADS * SSD_HEAD_DIM
SSD_GROUPS = 2
SSD_STATE = 64
SSD_CONV = 4
SSD_CHUNK = 128
SSD_CONV_CH = SSD_INNER + 2 * SSD_GROUPS * SSD_STATE
ATT_HEADS = 16
ATT_HEAD_DIM = 64
ATT_INNER = ATT_HEADS * ATT_HEAD_DIM
MOBA_BLOCK = 256
MOBA_TOPK = 3
MOBA_QCHUNK = 16
ROPE_THETA = 10000.0
IN_SPLITS = (SSD_INNER, SSD_CONV_CH, SSD_HEADS, ATT_INNER, ATT_INNER, ATT_INNER, D_MODEL, D_MODEL)
IN_WIDTH = 7440
N_GROUPS = 4
EXPERTS_PER_GROUP = 8
TOPK_IN_GROUP = 2
D_EXPERT = 256

kernel_name = "hybrid_ssd_moba_hiermoe_block"


def rms_norm(x, w):
    xf = x.astype(jnp.float32)
    inv = lax.rsqrt(jnp.mean(xf * xf, axis=-1, keepdims=True) + EPS)
    return (xf * inv).astype(x.dtype) * w


def split_columns(proj):
    offs = np.cumsum(IN_SPLITS)[:-1].tolist()
    return jnp.split(proj, offs, axis=-1)


def rope(x, positions):
    half = x.shape[-1] // 2
    inv_freq = ROPE_THETA ** (-jnp.arange(half, dtype=jnp.float32) / half)
    ang = positions.astype(jnp.float32)[..., None] * inv_freq
    cos = jnp.cos(ang)[:, :, None, :].astype(x.dtype)
    sin = jnp.sin(ang)[:, :, None, :].astype(x.dtype)
    x1, x2 = x[..., :half], x[..., half:]
    return jnp.concatenate([x1 * cos - x2 * sin, x2 * cos + x1 * sin], axis=-1)


def causal_dwconv(u, w, b):
    out = lax.conv_general_dilated(
        u, w[:, None, :].astype(u.dtype), window_strides=(1,),
        padding=((SSD_CONV - 1, 0),), dimension_numbers=("NWC", "WIO", "NWC"),
        feature_group_count=u.shape[-1])
    return out + b


def ssd_mixer(z, xbc, dt_raw, conv_w, conv_b, dt_bias, a_log, d_skip, norm_w):
    b, L, _ = z.shape
    nc = L // SSD_CHUNK
    E = SSD_HEADS // SSD_GROUPS
    xbc = jax.nn.silu(causal_dwconv(xbc, conv_w, conv_b))
    xs, Bm, Cm = jnp.split(xbc, [SSD_INNER, SSD_INNER + SSD_GROUPS * SSD_STATE], axis=-1)
    dt = jax.nn.softplus(dt_raw.astype(jnp.float32) + dt_bias.astype(jnp.float32))
    A = -jnp.exp(a_log.astype(jnp.float32)).reshape(SSD_GROUPS, E)
    X = xs.reshape(b, nc, SSD_CHUNK, SSD_GROUPS, E, SSD_HEAD_DIM)
    Bc = Bm.reshape(b, nc, SSD_CHUNK, SSD_GROUPS, SSD_STATE)
    Cc = Cm.reshape(b, nc, SSD_CHUNK, SSD_GROUPS, SSD_STATE)
    dtc = dt.reshape(b, nc, SSD_CHUNK, SSD_GROUPS, E)
    Xdt = X * dtc[..., None].astype(X.dtype)
    dA = jnp.transpose(dtc * A, (0, 1, 3, 4, 2))
    A_cum = jnp.cumsum(dA, axis=-1)
    idx = jnp.arange(SSD_CHUNK)
    causal = idx[:, None] >= idx[None, :]
    Lmat = jnp.exp(jnp.where(causal, A_cum[..., :, None] - A_cum[..., None, :], -jnp.inf))
    CB = jnp.einsum("bclgn,bcsgn->bcgls", Cc, Bc)
    M = (CB[:, :, :, None] * Lmat).astype(X.dtype)
    y_diag = jnp.einsum("bcgels,bcsgep->bclgep", M, Xdt)
    decay_states = jnp.exp(A_cum[..., -1:] - A_cum).astype(X.dtype)
    states = jnp.einsum("bclgn,bcgel,bclgep->bcgepn", Bc, decay_states, Xdt)
    chunk_decay = jnp.exp(A_cum[..., -1])

    def step(h, inp):
        s_c, d_c = inp
        return h * d_c[..., None, None] + s_c, h

    h0 = jnp.zeros((b, SSD_GROUPS, E, SSD_HEAD_DIM, SSD_STATE), jnp.float32)
    _, prev = lax.scan(step, h0, (jnp.moveaxis(states, 1, 0).astype(jnp.float32),
                                  jnp.moveaxis(chunk_decay, 1, 0)))
    prev = jnp.moveaxis(prev, 0, 1).astype(X.dtype)
    y_off = jnp.einsum("bclgn,bcgepn,bcgel->bclgep", Cc, prev, jnp.exp(A_cum).astype(X.dtype))
    y = y_diag + y_off + X * d_skip.reshape(SSD_GROUPS, E)[:, :, None].astype(X.dtype)
    y = y.reshape(b, L, SSD_INNER)
    yg = (y * jax.nn.silu(z)).reshape(b, L, SSD_GROUPS, SSD_INNER // SSD_GROUPS)
    return rms_norm(yg, norm_w.reshape(SSD_GROUPS, -1)).reshape(b, L, SSD_INNER)


def moba_attention(q, k, v):
    b, L, H, Dh = q.shape
    Lp = -(-L // MOBA_BLOCK) * MOBA_BLOCK
    nb = Lp // MOBA_BLOCK
    n_sel = min(MOBA_TOPK, nb - 1)
    pad = ((0, 0), (0, Lp - L), (0, 0), (0, 0))
    qh = jnp.pad(q, pad).transpose(0, 2, 1, 3)
    k_blocks = jnp.pad(k, pad).transpose(0, 2, 1, 3).reshape(b, H, nb, MOBA_BLOCK, Dh)
    v_blocks = jnp.pad(v, pad).transpose(0, 2, 1, 3).reshape(b, H, nb, MOBA_BLOCK, Dh)
    k_mean = jnp.mean(k_blocks.astype(jnp.float32), axis=3).astype(q.dtype)
    scale = Dh ** -0.5
    bi = jnp.arange(b)[:, None, None, None]
    hi = jnp.arange(H)[None, :, None, None]
    kpos_in_blk = jnp.arange(MOBA_BLOCK)

    def chunk(c):
        q0 = c * MOBA_QCHUNK
        qc = lax.dynamic_slice_in_dim(qh, q0, MOBA_QCHUNK, axis=2)
        qpos = q0 + jnp.arange(MOBA_QCHUNK)
        own = q0 // MOBA_BLOCK
        own_idx = jnp.broadcast_to(own, (b, H, MOBA_QCHUNK, 1)).astype(jnp.int32)
        if n_sel > 0:
            gate = jnp.einsum("bhqd,bhnd->bhqn", qc, k_mean).astype(jnp.float32)
            gate = jnp.where(jnp.arange(nb) < own, gate, -jnp.inf)
            _, sel = lax.top_k(gate, n_sel)
            sel = sel.astype(jnp.int32)
            blk_idx = jnp.concatenate([sel, own_idx], axis=-1)
            slot_ok = jnp.concatenate([sel < own, jnp.ones(own_idx.shape, bool)], axis=-1)
        else:
            blk_idx = own_idx
            slot_ok = jnp.ones(own_idx.shape, bool)
        kg = k_blocks[bi, hi, blk_idx]
        vg = v_blocks[bi, hi, blk_idx]
        s = jnp.einsum("bhqd,bhqjkd->bhqjk", qc, kg).astype(jnp.float32) * scale
        kpos = blk_idx[..., None] * MOBA_BLOCK + kpos_in_blk
        mask = slot_ok[..., None] & (kpos <= qpos[:, None, None])
        s = jnp.where(mask, s, -jnp.inf)
        pr = jax.nn.softmax(s.reshape(b, H, MOBA_QCHUNK, -1), axis=-1).reshape(s.shape)
        return jnp.einsum("bhqjk,bhqjkd->bhqd", pr.astype(vg.dtype), vg)

    out = lax.map(chunk, jnp.arange(Lp // MOBA_QCHUNK))
    out = out.transpose(1, 0, 3, 2, 4).reshape(b, Lp, H, Dh)
    return out[:, :L]


def hybrid_mixer(h, positions, w_in, conv_w, conv_b, dt_bias, a_log, d_skip, ssd_norm_w,
                 w_ssd_out, w_attn_out, w_out):
    b, L, _ = h.shape
    z, xbc, dt_raw, q, k, v, g_ssd, g_att = split_columns(h @ w_in)
    y_ssd = ssd_mixer(z, xbc, dt_raw, conv_w, conv_b, dt_bias, a_log, d_skip, ssd_norm_w) @ w_ssd_out
    q = rope(q.reshape(b, L, ATT_HEADS, ATT_HEAD_DIM), positions)
    k = rope(k.reshape(b, L, ATT_HEADS, ATT_HEAD_DIM), positions)
    v = v.reshape(b, L, ATT_HEADS, ATT_HEAD_DIM)
    y_att = moba_attention(q, k, v).reshape(b, L, ATT_INNER) @ w_attn_out
    merged = jax.nn.sigmoid(g_ssd) * y_ssd + jax.nn.sigmoid(g_att) * y_att
    return merged @ w_out


def hier_moe(h, w_rg, b_rg, w_re, b_re, w_gate, w_up, w_down):
    b, L, _ = h.shape
    gl = (h @ w_rg).astype(jnp.float32) + b_rg.astype(jnp.float32)
    gp = jax.nn.softmax(gl, axis=-1)
    _, gi = lax.top_k(gl, 1)
    g_oh = jax.nn.one_hot(gi[..., 0], N_GROUPS, dtype=jnp.float32)
    g_w = jnp.sum(gp * g_oh, axis=-1)
    el = ((h @ w_re).astype(jnp.float32) + b_re.astype(jnp.float32)).reshape(b, L, N_GROUPS, EXPERTS_PER_GROUP)
    el_sel = jnp.einsum("blge,blg->ble", el, g_oh)
    top_v, top_i = lax.top_k(el_sel, TOPK_IN_GROUP)
    top_w = jax.nn.softmax(top_v, axis=-1) * g_w[..., None]
    e_w = jnp.sum(jax.nn.one_hot(top_i, EXPERTS_PER_GROUP, dtype=jnp.float32) * top_w[..., None], axis=-2)
    comb = (g_oh[..., None] * e_w[..., None, :]).astype(h.dtype)
    out = jnp.zeros_like(h)
    for g in range(N_GROUPS):
        hid = jax.nn.silu(jnp.einsum("bld,edf->blef", h, w_gate[g])) * jnp.einsum("bld,edf->blef", h, w_up[g])
        out = out + jnp.einsum("blef,ble,efd->bld", hid, comb[:, :, g], w_down[g])
    return out


def setup_inputs(seed: int = 0) -> dict:
    key = jax.random.key(seed)
    ks = jax.random.split(key, 32)
    f32 = jnp.float32

    def nrm(k, shape, fan_in):
        return jax.random.normal(k, shape, f32) * (fan_in ** -0.5)

    def gain(k, shape):
        return 1.0 + 0.05 * jax.random.normal(k, shape, f32)

    dt0 = jnp.exp(jax.random.uniform(ks[5], (DEPTH, SSD_HEADS), f32, math.log(1e-3), math.log(1e-1)))
    dt_bias = dt0 + jnp.log(-jnp.expm1(-dt0))
    return {
        "x": jax.random.normal(ks[0], (BATCH, SEQ, D_MODEL), f32),
        "p": jax.random.normal(ks[1], (DEPTH, BATCH, SEQ, PLE_DIM), f32),
        "positions": (jnp.arange(SEQ, dtype=jnp.int32)[None, :]
                      + jax.random.randint(ks[2], (BATCH, 1), 0, 1024, dtype=jnp.int32)),
        "attn_norm_w": gain(ks[3], (DEPTH, D_MODEL)),
        "w_in": nrm(ks[4], (DEPTH, D_MODEL, IN_WIDTH), D_MODEL),
        "conv_w": nrm(ks[6], (DEPTH, SSD_CONV, SSD_CONV_CH), SSD_CONV),
        "conv_b": 0.02 * jax.random.normal(ks[7], (DEPTH, SSD_CONV_CH), f32),
        "dt_bias": dt_bias,
        "a_log": jnp.log(jax.random.uniform(ks[8], (DEPTH, SSD_HEADS), f32, 1.0, 16.0)),
        "d_skip": 1.0 + 0.1 * jax.random.normal(ks[9], (DEPTH, SSD_HEADS), f32),
        "ssd_norm_w": gain(ks[10], (DEPTH, SSD_INNER)),
        "w_ssd_out": nrm(ks[11], (DEPTH, SSD_INNER, D_MODEL), SSD_INNER),
        "w_attn_out": nrm(ks[12], (DEPTH, ATT_INNER, D_MODEL), ATT_INNER),
        "w_out": nrm(ks[13], (DEPTH, D_MODEL, D_MODEL), D_MODEL),
        "moe_norm_w": gain(ks[14], (DEPTH, D_MODEL)),
        "w_router_group": nrm(ks[15], (DEPTH, D_MODEL, N_GROUPS), D_MODEL),
        "b_router_group": 0.01 * jax.random.normal(ks[16], (DEPTH, N_GROUPS), f32),
        "w_router_expert": nrm(ks[17], (DEPTH, D_MODEL, N_GROUPS * EXPERTS_PER_GROUP), D_MODEL),
        "b_router_expert": 0.01 * jax.random.normal(ks[18], (DEPTH, N_GROUPS * EXPERTS_PER_GROUP), f32),
        "w_exp_gate": nrm(ks[19], (DEPTH, N_GROUPS, EXPERTS_PER_GROUP, D_MODEL, D_EXPERT), D_MODEL),
        "w_exp_up": nrm(ks[20], (DEPTH, N_GROUPS, EXPERTS_PER_GROUP, D_MODEL, D_EXPERT), D_MODEL),
        "w_exp_down": nrm(ks[21], (DEPTH, N_GROUPS, EXPERTS_PER_GROUP, D_EXPERT, D_MODEL), D_EXPERT),
        "ple_norm_w": gain(ks[22], (DEPTH, D_MODEL)),
        "w_ple": nrm(ks[23], (DEPTH, PLE_DIM, D_MODEL), PLE_DIM),
        "w_ple_gate": nrm(ks[24], (DEPTH, D_MODEL, D_MODEL), D_MODEL),
        "final_norm_w": gain(ks[25], (D_MODEL,)),
    }


def reference(x, p, positions, attn_norm_w, w_in, conv_w, conv_b, dt_bias, a_log, d_skip,
              ssd_norm_w, w_ssd_out, w_attn_out, w_out, moe_norm_w, w_router_group,
              b_router_group, w_router_expert, b_router_expert, w_exp_gate, w_exp_up,
              w_exp_down, ple_norm_w, w_ple, w_ple_gate, final_norm_w):
    for i in range(DEPTH):
        h = rms_norm(x, attn_norm_w[i])
        x = x + hybrid_mixer(h, positions, w_in[i], conv_w[i], conv_b[i], dt_bias[i], a_log[i],
                             d_skip[i], ssd_norm_w[i], w_ssd_out[i], w_attn_out[i], w_out[i])
        h = rms_norm(x, moe_norm_w[i])
        x = x + hier_moe(h, w_router_group[i], b_router_group[i], w_router_expert[i],
                         b_router_expert[i], w_exp_gate[i], w_exp_up[i], w_exp_down[i])
        gate = jax.nn.sigmoid(rms_norm(x, ple_norm_w[i]) @ w_ple_gate[i])
        x = x + gate * (p[i] @ w_ple[i])
    return rms_norm(x, final_norm_w)
```

```python
import bisect
import math
from contextlib import ExitStack

import numpy as np
import concourse.bass as bass
import concourse.mybir as mybir
from concourse.bass_utils import run_bass_kernel_spmd

F32 = mybir.dt.float32
BF16 = mybir.dt.bfloat16
I32 = mybir.dt.int32
AF = mybir.ActivationFunctionType
ALU = mybir.AluOpType
AX = mybir.AxisListType

ENGS = ("pe", "act", "dve", "pool", "sp")
D = 1024
KT = 8
EPS = 1e-6
BIG = 32768.0
PI = math.pi


class Sched:
    def __init__(self, nc, n_dma_ch=40):
        self.nc = nc
        self.ops = {e: [] for e in ENGS}
        self.sig = {e: [] for e in ENGS}
        self.flushed = {e: 0 for e in ENGS}
        self.res = {}
        self.waited = {e: {} for e in ENGS}
        self.sems = {}
        self.dma_cnt = {}
        self.dma_last = {}
        self.n_dma_ch = n_dma_ch
        self.rr = 0
        self.last_real = {e: None for e in ENGS}

    def alloc_sems(self, stack):
        for e in ENGS:
            self.sems[e] = stack.enter_context(self.nc.semaphore("s_" + e))
        for i in range(self.n_dma_ch):
            k = "d%d" % i
            self.sems[k] = stack.enter_context(self.nc.semaphore("s_" + k))
            self.dma_cnt[k] = 0
            self.dma_last[k] = None

    def _resolve(self, tok):
        if tok[0] == "d":
            return tok[1], tok[2]
        _, e, i = tok
        lst = self.sig[e]
        j = bisect.bisect_left(lst, i)
        if j == len(lst):
            last = self.last_real[e]
            assert last is not None and last >= i and last >= self.flushed[e], (e, i, last)
            self.ops[e][last]["sig"] = True
            lst.append(last)
        return e, j + 1

    def mark(self, i):
        pass

    def emit(self, eng, fn, reads=(), writes=(), dma_ch=None):
        if getattr(self, "drop", False):
            return None
        deps = []
        for r in reads:
            st = self.res.get(r)
            if st and st[0] is not None:
                deps.append((st[0], True))
        for w in writes:
            st = self.res.get(w)
            if st:
                if st[0] is not None:
                    deps.append((st[0], False))
                for rd in st[1]:
                    deps.append((rd, False))
        is_dma = dma_ch is not None
        if is_dma and self.dma_last[dma_ch] is not None:
            deps.append((self.dma_last[dma_ch], True))
        waits = {}
        for tok, raw in deps:
            if tok[0] == "c" and tok[1] == eng and not is_dma and not raw:
                continue
            k, v = self._resolve(tok)
            if self.waited[eng].get(k, 0) >= v:
                continue
            if waits.get(k, 0) < v:
                waits[k] = v
        wme = self.waited[eng]
        final = []
        for k, v in sorted(waits.items(), key=lambda kv: (kv[0] not in ("pe", "act", "dve"), -kv[1])):
            if wme.get(k, 0) >= v:
                continue
            final.append((k, v))
            wme[k] = v
            if k in ("pe", "act", "dve") and k != eng:
                snap = self.ops[k][self.sig[k][v - 1]].get("vc")
                if snap:
                    for kk, vv in snap.items():
                        if wme.get(kk, 0) < vv:
                            wme[kk] = vv
        waits = dict(final)
        idx = len(self.ops[eng])
        rec = {"fn": fn, "waits": final, "sig": False, "dma": None}
        if not is_dma and eng in ("pe", "act", "dve"):
            rec["vc"] = dict(wme)
        if is_dma:
            self.dma_cnt[dma_ch] += 16
            me = ("d", dma_ch, self.dma_cnt[dma_ch])
            rec["dma"] = dma_ch
            self.dma_last[dma_ch] = me
        else:
            me = ("c", eng, idx)
            self.last_real[eng] = idx
        self.ops[eng].append(rec)
        for r in reads:
            self.res.setdefault(r, [None, []])[1].append(me)
        for w in writes:
            self.res[w] = [me, []]
        return me

    def ch(self):
        k = "d%d" % (self.rr % self.n_dma_ch)
        self.rr += 1
        return k

    def barrier(self):
        toks = []
        for e in ENGS:
            if self.last_real[e] is not None:
                toks.append(("c", e, self.last_real[e]))
        for k, t in self.dma_last.items():
            if t is not None:
                toks.append(t)
        res = [self._resolve(t) for t in toks]
        for e in ENGS:
            waits = {}
            for k, v in res:
                if k == e:
                    continue
                if self.waited[e].get(k, 0) >= v:
                    continue
                waits[k] = max(waits.get(k, 0), v)
            for k, v in waits.items():
                self.waited[e][k] = v
            if waits:
                self.ops[e].append({"fn": None, "waits": list(waits.items()),
                                    "sig": False, "dma": None})
        self.res = {}

    def flush(self):
        nc = self.nc
        S = self

        def replay(name, eng):
            ops = S.ops[name]
            for rec in ops[S.flushed[name]:]:
                waits = rec["waits"]
                fuse = rec["fn"] is not None and len(waits) > 0
                for k, v in (waits[:-1] if fuse else waits):
                    eng.wait_ge(S.sems[k], v)
                if rec["fn"] is None:
                    continue
                ins = rec["fn"](eng)
                if fuse:
                    ins._wait_ge(S.sems[waits[-1][0]], waits[-1][1])
                if rec["dma"] is not None:
                    ins.then_inc(S.sems[rec["dma"]], 16)
                elif rec["sig"]:
                    ins.then_inc(S.sems[name], 1)
            S.flushed[name] = len(ops)

        with nc.Block() as block:
            @block.tensor
            def _(e):
                replay("pe", e)

            @block.scalar
            def _(e):
                replay("act", e)

            @block.vector
            def _(e):
                replay("dve", e)

            @block.gpsimd
            def _(e):
                replay("pool", e)

            @block.sync
            def _(e):
                replay("sp", e)


OFF_Z, OFF_XBC, OFF_DT, OFF_Q, OFF_K, OFF_V, OFF_GS, OFF_GA = 0, 1024, 2304, 2320, 3344, 4368, 5392, 6416


def host_consts(L=4096):
    c = {}
    c["ident"] = np.eye(128, dtype=np.float32)
    i = np.arange(128)
    c["T32"] = (i[:, None] <= i[None, :]).astype(np.float32)
    c["U32"] = (i[:, None] > i[None, :]).astype(np.float32)
    half = 32
    inv = (10000.0 ** (-np.arange(half, dtype=np.float32) / half)).astype(np.float32)
    c["invf"] = inv[i % 32].reshape(128, 1).astype(np.float32)
    c["sgn"] = np.where((i % 64) < 32, -1.0, 1.0).reshape(128, 1).astype(np.float32)
    pm = np.zeros((128, 128), np.float32)
    for p in range(128):
        src = p + 32 if (p % 64) < 32 else p - 32
        pm[src, p] = 1.0
    c["perm"] = pm
    oh = np.zeros((16, 16, 128), np.float32)
    for n in range(16):
        oh[n, n, :] = 1.0
    c["oh"] = oh.reshape(16, 16 * 128)
    kk = np.arange(4096)
    c["blkoh"] = (kk[None, :] // 256 == np.arange(16)[:, None]).astype(np.float32)
    NT = L // 128
    own = (np.arange(NT) // 2)[:, None, None]
    nn = np.arange(16)[None, None, :]
    z2 = np.zeros((NT, 2, 16), np.float32)
    c["gA"] = (z2 + np.where(nn >= own, -1e30, 0.0)).reshape(1, NT * 32).astype(np.float32)
    c["gM"] = (z2 + np.where(nn < own, 1.0, 0.0)).reshape(1, NT * 32).astype(np.float32)
    c["gO"] = (z2 + np.where(nn == own, 1.0, 0.0)).reshape(1, NT * 32).astype(np.float32)
    cm = np.zeros((128, 4, 512), np.float32)
    for r in range(4):
        for j in range(512):
            same = (r < 2) == (j < 256)
            kl = r * 128 + i
            cm[:, r, j] = np.where(same & (kl > j), -BIG, 0.0)
    c["cmask"] = cm.reshape(128, 2048)
    return c


def build(L, upto=99, debug=()):
    NT = L // 128
    NCH = L // 512
    NB = L // 256
    nc = bass.Bass("TRN2", target_bir_lowering=False)
    dbg_out = {}

    def din(name, shape, dt=F32):
        return nc.dram_tensor(name, shape, dt, kind="ExternalInput").ap()

    def dscr(name, shape, dt):
        kind = "ExternalOutput" if name in debug else "Internal"
        t = nc.dram_tensor(name, shape, dt, kind=kind).ap()
        if name in debug:
            dbg_out[name] = t
        return t

    x_d = din("x", [L, D])
    p_d = din("p", [L, 256])
    pos_d = din("pos", [1, L], I32)
    w_in_d = din("w_in", [D, 7440])
    nw1_d = din("attn_norm_w", [1, D])
    cw_d = din("conv_w", [128, 40])
    cb_d = din("conv_b", [128, 10])
    dtb_d = din("dt_bias", [1, 16])
    alog_d = din("a_log", [1, 16])
    dsk_d = din("d_skip", [1, 16])
    snw_d = din("ssd_norm_w", [1, D])
    wso_d = din("w_ssd_out", [D, D])
    wao_d = din("w_attn_out", [D, D])
    wo_d = din("w_out", [D, D])
    nw2_d = din("moe_norm_w", [1, D])
    wrt_d = din("w_rt", [D, 36])
    brt_d = din("b_rt", [1, 36])
    wg_d = din("w_exp_gate", [32, D, 256])
    wu_d = din("w_exp_up", [32, D, 256])
    wd_d = din("w_exp_down", [32, 256, D])
    nw3_d = din("ple_norm_w", [1, D])
    wple_d = din("w_ple", [256, D])
    wpg_d = din("w_ple_gate", [D, D])
    nwf_d = din("final_norm_w", [1, D])
    c_ident_d = din("c_ident", [128, 128])
    c_T_d = din("c_T32", [128, 128])
    c_U_d = din("c_U32", [128, 128])
    c_invf_d = din("c_invf", [128, 1])
    c_sgn_d = din("c_sgn", [128, 1])
    c_perm_d = din("c_perm", [128, 128])
    c_oh_d = din("c_oh", [16, 2048])
    c_blkoh_d = din("c_blkoh", [16, 4096])
    c_gA_d = din("c_gA", [1, NT * 32])
    c_gM_d = din("c_gM", [1, NT * 32])
    c_gO_d = din("c_gO", [1, NT * 32])
    c_cm_d = din("c_cmask", [128, 2048])
    out_d = nc.dram_tensor("out", [L, D], F32, kind="ExternalOutput").ap()

    xbcT_s = dscr("xbcT_s", [1280, L], BF16)
    qT_s = dscr("qT_s", [D, L], BF16)
    kT_s = dscr("kT_s", [D, L], BF16)
    sgT_s = dscr("sgT_s", [2 * D, L], BF16)
    zs_s = dscr("zs_s", [L, D], BF16)
    vaug_s = dscr("vaug_s", [L, 16 * 65], BF16)
    dt_s = dscr("dt_s", [128, NT * 16], F32)
    ssdT_s = dscr("ssdT_s", [D, L], BF16)
    attT_s = dscr("attT_s", [D, L], BF16)
    x1_s = dscr("x1_s", [L, D], F32)

    top = ExitStack()
    with top:
        S = Sched(nc)
        S.alloc_sems(top)
        lp = top.enter_context(nc.allow_low_precision("bf16 matmul operands, fp32 accumulation"))

        uniq = [0]

        def sbt(st, name, shape, dt):
            uniq[0] += 1
            return st.enter_context(nc.sbuf_tensor("%s_u%d" % (name, uniq[0]), shape, dt))

        def pst(st, name, shape, dt):
            uniq[0] += 1
            return st.enter_context(nc.psum_tensor("%s_u%d" % (name, uniq[0]), shape, dt))

        def dma(q, out, in_, reads=(), writes=()):
            return S.emit(q, lambda e: e.dma_start(out=out, in_=in_), reads=reads, writes=writes, dma_ch=S.ch())

        ident_f = sbt(top, "ident_f", [128, 128], F32)
        ident_b = sbt(top, "ident_b", [128, 128], BF16)
        dtall = sbt(top, "dtall", [128, NT, 16], F32)
        dma("sp", ident_f[:], c_ident_d, writes=["ident_f"])
        dma("pool", ident_b[:], c_ident_d, writes=["ident_b"])

        def rstd_from_ss(ss_ap, rstd_ap, n, keys_r, keys_w):
            S.emit("act", lambda e: e.activation(out=rstd_ap, in_=ss_ap, func=AF.Sqrt, scale=1.0 / n, bias=EPS_T[:, 0:1]),
                   reads=list(keys_r) + ["eps_t"], writes=keys_w)
            S.emit("dve", lambda e: e.reciprocal(out=rstd_ap, in_=rstd_ap), reads=keys_w, writes=keys_w)

        NHALF = sbt(top, "nhalf", [128, 1], F32)
        S.emit("pool", lambda e: e.memset(NHALF[:], -0.5), writes=["nhalf"])

        def rstd_pow(ss_ap, rstd_ap, n, keys_r, keys_w):
            S.emit("dve", lambda e: e.tensor_scalar(out=rstd_ap, in0=ss_ap, scalar1=1.0 / n, scalar2=EPS, op0=ALU.mult, op1=ALU.add),
                   reads=list(keys_r), writes=keys_w)
            S.emit("pool", lambda e: e.tensor_tensor(out=rstd_ap, in0=rstd_ap, in1=NHALF[:, 0:1], op=ALU.pow), reads=keys_w + ["nhalf"], writes=keys_w)

        EPS_T = sbt(top, "eps_t", [128, 1], F32)
        S.emit("pool", lambda e: e.memset(EPS_T[:], EPS), writes=["eps_t"])

        with ExitStack() as ph:
            hT = sbt(ph, "hT", [128, KT, L], BF16)
            cosT = sbt(ph, "cosT", [128, L], F32)
            sinT = sbt(ph, "sinT", [128, L], F32)
            rtmp = sbt(ph, "rtmp", [128, L], F32)
            rtmp2 = sbt(ph, "rtmp2", [128, L], F32)
            rti = sbt(ph, "rti", [128, L], I32)
            nwbc = sbt(ph, "nwbc", [128, D], F32)
            invf = sbt(ph, "invf", [128, 1], F32)
            sgn = sbt(ph, "sgn", [128, 1], F32)
            perm = sbt(ph, "perm", [128, 128], BF16)
            xin = [sbt(ph, "xin%d" % i, [128, D], F32) for i in range(3)]
            hn = [sbt(ph, "hn%d" % i, [128, D], BF16) for i in range(3)]
            junk = sbt(ph, "junk", [128, D], BF16)
            ss = [sbt(ph, "ss%d" % i, [128, 1], F32) for i in range(3)]
            rs = [sbt(ph, "rs%d" % i, [128, 1], F32) for i in range(3)]
            wbf = [sbt(ph, "wbf%d" % i, [128, KT, 512], BF16) for i in range(2)]
            stg = [sbt(ph, "stg%d" % i, [128, 512], BF16) for i in range(4)]
            vst = [sbt(ph, "vst%d" % i, [128, 8, 65], BF16) for i in range(2)]
            qsb = [sbt(ph, "qsb%d" % i, [128, 512], BF16) for i in range(2)]
            t1 = [sbt(ph, "t1_%d" % i, [128, 512], F32) for i in range(2)]
            t2 = [sbt(ph, "t2_%d" % i, [128, 512], F32) for i in range(2)]
            pp = [pst(ph, "pp%d" % i, [128, 512], F32) for i in range(4)]
            pT = [pst(ph, "pT%d" % i, [128, KT, 128], BF16) for i in range(2)]
            prot = [pst(ph, "prot%d" % i, [128, 512], F32) for i in range(2)]

            dma("sp", nwbc[:], nw1_d.partition_broadcast(128), writes=["nwbc"])
            dma("sp", invf[:], c_invf_d, writes=["invf"])
            dma("sp", sgn[:], c_sgn_d, writes=["sgn"])
            dma("pool", perm[:], c_perm_d, writes=["perm"])
            dma("sp", rti[:], pos_d.partition_broadcast(128), writes=["rti"])
            for i in range(2):
                S.emit("pool", lambda e, i=i: e.memset(vst[i][:, :, 64:65], 1.0), writes=["vst%d" % i])

            def n1_s1(t):
                s = t % 3
                dma("sp", xin[s][:], x_d[t * 128:(t + 1) * 128, :], writes=["xin%d" % s])
                S.emit("act", lambda e, s=s: e.activation(out=junk[:], in_=xin[s][:], func=AF.Square, accum_out=ss[s][:]),
                       reads=["xin%d" % s], writes=["junk", "ss%d" % s])
                rstd_from_ss(ss[s][:], rs[s][:], D, ["ss%d" % s], ["rs%d" % s])
                S.emit("dve", lambda e, s=s: e.scalar_tensor_tensor(out=hn[s][:], in0=xin[s][:], scalar=rs[s][:, 0:1], in1=nwbc[:],
                                                                   op0=ALU.mult, op1=ALU.mult),
                       reads=["xin%d" % s, "rs%d" % s, "nwbc"], writes=["hn%d" % s])

            def n1_s2(t):
                s = t % 3
                u = t % 2
                for k in range(KT):
                    S.emit("pe", lambda e, s=s, k=k, u=u: e.transpose(pT[u][:, k, :], hn[s][:, k * 128:(k + 1) * 128], ident_b[:]),
                           reads=["hn%d" % s, "ident_b"], writes=["pT%d" % u])
                S.emit("act", lambda e, u=u, t=t: e.copy(out=hT[:, :, t * 128:(t + 1) * 128], in_=pT[u][:]),
                       reads=["pT%d" % u], writes=[("hT", t)])

            for t in range(NT + 1):
                if t < NT:
                    n1_s1(t)
                if t >= 1:
                    n1_s2(t - 1)

            S.emit("dve", lambda e: e.tensor_copy(out=rtmp[:], in_=rti[:]), reads=["rti"], writes=["rtmp"])
            S.emit("dve", lambda e: e.tensor_scalar(out=rtmp[:], in0=rtmp[:], scalar1=invf[:, 0:1], scalar2=None, op0=ALU.mult),
                   reads=["rtmp", "invf"], writes=["rtmp"])

            def sin_table(dst, shift, key):
                S.emit("dve", lambda e: e.tensor_scalar(out=rtmp2[:], in0=rtmp[:], scalar1=shift, scalar2=1.0 / (2 * PI),
                                                        op0=ALU.add, op1=ALU.mult), reads=["rtmp"], writes=["rtmp2"])
                S.emit("dve", lambda e: e.tensor_copy(out=rti[:], in_=rtmp2[:]), reads=["rtmp2"], writes=["rti"])
                S.emit("dve", lambda e: e.tensor_copy(out=rtmp2[:], in_=rti[:]), reads=["rti"], writes=["rtmp2"])
                S.emit("dve", lambda e: e.tensor_scalar(out=rtmp2[:], in0=rtmp2[:], scalar1=-2 * PI, scalar2=shift,
                                                        op0=ALU.mult, op1=ALU.add), reads=["rtmp2"], writes=["rtmp2"])
                S.emit("dve", lambda e: e.tensor_tensor(out=rtmp2[:], in0=rtmp2[:], in1=rtmp[:], op=ALU.add),
                       reads=["rtmp2", "rtmp"], writes=["rtmp2"])
                S.emit("dve", lambda e: e.tensor_scalar(out=dst[:], in0=rtmp2[:], scalar1=PI, scalar2=-2 * PI,
                                                        op0=ALU.is_gt, op1=ALU.mult), reads=["rtmp2"], writes=[key])
                S.emit("dve", lambda e: e.tensor_tensor(out=rtmp2[:], in0=rtmp2[:], in1=dst[:], op=ALU.add),
                       reads=["rtmp2", key], writes=["rtmp2"])
                S.emit("dve", lambda e: e.tensor_scalar(out=dst[:], in0=rtmp2[:], scalar1=-PI, scalar2=2 * PI,
                                                        op0=ALU.is_lt, op1=ALU.mult), reads=["rtmp2"], writes=[key])
                S.emit("dve", lambda e: e.tensor_tensor(out=rtmp2[:], in0=rtmp2[:], in1=dst[:], op=ALU.add),
                       reads=["rtmp2", key], writes=["rtmp2"])
                S.emit("dve", lambda e: e.tensor_scalar(out=rtmp2[:], in0=rtmp2[:], scalar1=PI, scalar2=-PI,
                                                        op0=ALU.min, op1=ALU.max), reads=["rtmp2"], writes=["rtmp2"])
                S.emit("act", lambda e: e.activation(out=dst[:], in_=rtmp2[:], func=AF.Sin), reads=["rtmp2"], writes=[key])

            sin_table(cosT, PI / 2, "cosT")
            sin_table(sinT, 0.0, "sinT")
            S.emit("dve", lambda e: e.tensor_scalar(out=sinT[:], in0=sinT[:], scalar1=sgn[:, 0:1], scalar2=None, op0=ALU.mult),
                   reads=["sinT", "sgn"], writes=["sinT"])

            st_i = [0]
            pp_i = [0]
            w_i = [0]

            def load_w(c0, w):
                s = w_i[0] % 2
                w_i[0] += 1
                dma("pool", wbf[s][:, :, 0:w], w_in_d[:, c0:c0 + w].rearrange("(k p) c -> p k c", p=128), writes=["wbf%d" % s])
                return s

            def mm_fm(ws, j, n):
                ps = pp_i[0] % 4
                pp_i[0] += 1
                for k in range(KT):
                    S.emit("pe", lambda e, k=k, ps=ps: e.matmul(pp[ps][:], lhsT=wbf[ws][:, k, j * 128:(j + 1) * 128],
                                                               rhs=hT[:, k, n * 512:(n + 1) * 512], start=(k == 0), stop=(k == KT - 1)),
                           reads=["wbf%d" % ws] + [("hT", 4 * n + i) for i in range(4)], writes=["pp%d" % ps])
                return ps

            def next_stg():
                s = st_i[0] % 4
                st_i[0] += 1
                return s

            fm_segs = [("xbc", OFF_XBC, 1280), ("q", OFF_Q, 1024), ("k", OFF_K, 1024), ("gs", OFF_GS, 1024), ("ga", OFF_GA, 1024)]
            ev_i = [0]
            for name, off, width in fm_segs:
                for b0 in range(0, width, 512):
                    bw = min(512, width - b0)
                    ws = load_w(off + b0, bw)
                    for j in range(bw // 128):
                        row0 = b0 + j * 128
                        for n in range(NCH):
                            ps = mm_fm(ws, j, n)
                            cols = slice(n * 512, (n + 1) * 512)
                            if name == "xbc":
                                sg = next_stg()
                                eng = "dve" if ev_i[0] % 2 == 0 else "act"
                                ev_i[0] += 1
                                if eng == "dve":
                                    S.emit("dve", lambda e, sg=sg, ps=ps: e.tensor_copy(out=stg[sg][:], in_=pp[ps][:]),
                                           reads=["pp%d" % ps], writes=["stg%d" % sg])
                                else:
                                    S.emit("act", lambda e, sg=sg, ps=ps: e.copy(out=stg[sg][:], in_=pp[ps][:]),
                                           reads=["pp%d" % ps], writes=["stg%d" % sg])
                                dma("sp", xbcT_s[row0:row0 + 128, cols], stg[sg][:], reads=["stg%d" % sg], writes=[("xbcT", row0 // 128)])
                            elif name in ("gs", "ga"):
                                sg = next_stg()
                                S.emit("act", lambda e, sg=sg, ps=ps: e.activation(out=stg[sg][:], in_=pp[ps][:], func=AF.Sigmoid),
                                       reads=["pp%d" % ps], writes=["stg%d" % sg])
                                r0 = (0 if name == "gs" else D) + row0
                                dma("sp", sgT_s[r0:r0 + 128, cols], stg[sg][:], reads=["stg%d" % sg], writes=[("sgT", r0 // 128)])
                            else:
                                qs = ev_i[0] % 2
                                ev_i[0] += 1
                                S.emit("act", lambda e, qs=qs, ps=ps: e.copy(out=qsb[qs][:], in_=pp[ps][:]),
                                       reads=["pp%d" % ps], writes=["qsb%d" % qs])
                                S.emit("pe", lambda e, qs=qs: e.matmul(prot[qs][:], lhsT=perm[:], rhs=qsb[qs][:], start=True, stop=True),
                                       reads=["perm", "qsb%d" % qs], writes=["prot%d" % qs])
                                S.emit("pool", lambda e, qs=qs, cols=cols: e.tensor_tensor(out=t1[qs][:], in0=qsb[qs][:], in1=cosT[:, cols], op=ALU.mult),
                                       reads=["qsb%d" % qs, "cosT"], writes=["t1_%d" % qs])
                                S.emit("dve", lambda e, qs=qs, cols=cols: e.tensor_tensor(out=t2[qs][:], in0=prot[qs][:], in1=sinT[:, cols], op=ALU.mult),
                                       reads=["prot%d" % qs, "sinT"], writes=["t2_%d" % qs])
                                sg = next_stg()
                                S.emit("dve", lambda e, qs=qs, sg=sg: e.tensor_tensor(out=stg[sg][:], in0=t1[qs][:], in1=t2[qs][:], op=ALU.add),
                                       reads=["t1_%d" % qs, "t2_%d" % qs], writes=["stg%d" % sg])
                                dst = qT_s if name == "q" else kT_s
                                dma("sp", dst[row0:row0 + 128, cols], stg[sg][:], reads=["stg%d" % sg], writes=[(name + "T", row0 // 128)])

            for name, off, width in (("z", OFF_Z, 1024), ("v", OFF_V, 1024), ("dt", OFF_DT, 16)):
                for b0 in range(0, width, 512):
                    bw = min(512, width - b0)
                    ws = load_w(off + b0, bw)
                    for t in range(NT):
                        ps = pp_i[0] % 4
                        pp_i[0] += 1
                        for k in range(KT):
                            S.emit("pe", lambda e, k=k, ps=ps, t=t, bw=bw, ws=ws: e.matmul(
                                pp[ps][:, 0:bw], lhsT=hT[:, k, t * 128:(t + 1) * 128], rhs=wbf[ws][:, k, 0:bw],
                                start=(k == 0), stop=(k == KT - 1)),
                                reads=["wbf%d" % ws, ("hT", t)], writes=["pp%d" % ps])
                        rows = slice(t * 128, (t + 1) * 128)
                        if name == "z":
                            sg = next_stg()
                            S.emit("act", lambda e, sg=sg, ps=ps: e.activation(out=stg[sg][:], in_=pp[ps][:], func=AF.Silu),
                                   reads=["pp%d" % ps], writes=["stg%d" % sg])
                            dma("sp", zs_s[rows, b0:b0 + 512], stg[sg][:], reads=["stg%d" % sg], writes=[("zs", t)])
                        elif name == "v":
                            vs = ev_i[0] % 2
                            ev_i[0] += 1
                            S.emit("dve", lambda e, vs=vs, ps=ps: e.tensor_copy(out=vst[vs][:, :, 0:64], in_=pp[ps][:].rearrange("p (h d) -> p h d", h=8)),
                                   reads=["pp%d" % ps], writes=["vst%d" % vs])
                            h0 = b0 // 64
                            dma("sp", vaug_s[rows, h0 * 65:(h0 + 8) * 65], vst[vs][:].rearrange("p h d -> p (h d)"),
                                reads=["vst%d" % vs], writes=[("vaug", t)])
                        else:
                            S.emit("dve", lambda e, ps=ps, t=t: e.tensor_copy(out=dtall[:, t, :], in_=pp[ps][:, 0:16]),
                                   reads=["pp%d" % ps], writes=["dtall"])
            if "dt_s" in debug:
                dma("sp", dt_s, dtall[:].rearrange("p t h -> p (t h)"), reads=["dtall"], writes=["dt_s"])
            S.barrier()
            S.flush()


        if upto >= 2:
            with ExitStack() as ph:
                xc = sbt(ph, "xc", [128, 10, L], BF16)
                T32 = sbt(ph, "T32", [128, 128], F32)
                U32 = sbt(ph, "U32", [128, 128], F32)
                ones32 = sbt(ph, "ones32", [128, 128], F32)
                ONE_T = sbt(ph, "one_t", [128, 1], F32)
                cw = sbt(ph, "cw", [128, 40], F32)
                cb = sbt(ph, "cb", [128, 10], F32)
                dtb_bc = sbt(ph, "dtb_bc", [128, 16], F32)
                aneg = sbt(ph, "aneg", [128, 16], F32)
                dsk_bc = sbt(ph, "dsk_bc", [128, 16], F32)
                snwbc = sbt(ph, "snwbc", [128, D], F32)
                dx = sbt(ph, "dx", [128, NT, 16], F32)
                dl = sbt(ph, "dl", [128, NT, 16], F32)
                dtv = sbt(ph, "dtv", [128, NT, 16], F32)
                dA = sbt(ph, "dA", [128, NT, 16], F32)
                dma("sp", T32[:], c_T_d, writes=["T32"])
                dma("sp", U32[:], c_U_d, writes=["U32"])
                dma("sp", cw[:], cw_d, writes=["cw"])
                dma("sp", cb[:], cb_d, writes=["cb"])
                dma("sp", dtb_bc[:], dtb_d.partition_broadcast(128), writes=["dtb_bc"])
                dma("sp", aneg[:], alog_d.partition_broadcast(128), writes=["aneg"])
                dma("sp", dsk_bc[:], dsk_d.partition_broadcast(128), writes=["dsk_bc"])
                dma("sp", snwbc[:], snw_d.partition_broadcast(128), writes=["snwbc"])
                S.emit("pool", lambda e: e.memset(ones32[:], 1.0), writes=["ones32"])
                S.emit("pool", lambda e: e.memset(ONE_T[:], 1.0), writes=["one_t"])
                S.emit("dve", lambda e: e.tensor_tensor(out=dx[:], in0=dtall[:], in1=dtb_bc[:].unsqueeze(1).to_broadcast([128, NT, 16]), op=ALU.add),
                       reads=["dtall", "dtb_bc"], writes=["dx"])
                S.emit("dve", lambda e: e.scalar_tensor_tensor(out=dl[:], in0=dx[:], scalar=-1.0, in1=dx[:], op0=ALU.mult, op1=ALU.max), reads=["dx"], writes=["dl"])
                S.emit("act", lambda e: e.activation(out=dl[:], in_=dl[:], func=AF.Exp, scale=-1.0), reads=["dl"], writes=["dl"])
                S.emit("act", lambda e: e.activation(out=dl[:], in_=dl[:], func=AF.Ln, bias=ONE_T[:, 0:1]), reads=["dl", "one_t"], writes=["dl"])
                S.emit("dve", lambda e: e.scalar_tensor_tensor(out=dtv[:], in0=dx[:], scalar=0.0, in1=dl[:], op0=ALU.max, op1=ALU.add),
                       reads=["dx", "dl"], writes=["dtv"])
                S.emit("act", lambda e: e.activation(out=aneg[:], in_=aneg[:], func=AF.Exp), reads=["aneg"], writes=["aneg"])
                S.emit("dve", lambda e: e.tensor_scalar(out=aneg[:], in0=aneg[:], scalar1=-1.0, scalar2=None, op0=ALU.mult), reads=["aneg"], writes=["aneg"])
                S.emit("dve", lambda e: e.tensor_tensor(out=dA[:], in0=dtv[:], in1=aneg[:].unsqueeze(1).to_broadcast([128, NT, 16]), op=ALU.mult),
                       reads=["dtv", "aneg"], writes=["dA"])

                with ExitStack() as ph1:
                    cin = [sbt(ph1, "cin%d" % i, [128, L + 3], BF16) for i in range(2)]
                    dg = [sbt(ph1, "dg%d" % i, [128, 4, 128], BF16) for i in range(2)]
                    pc = [pst(ph1, "pc%d" % i, [128, 512], F32) for i in range(4)]
                    for i in range(2):
                        S.emit("pool", lambda e, i=i: e.memset(cin[i][:, 0:3], 0.0), writes=["cin%d" % i])
                    pi = 0
                    for ct in range(10):
                        s = ct % 2
                        dma("sp", cin[s][:, 3:3 + L], xbcT_s[ct * 128:(ct + 1) * 128, :], reads=[("xbcT", ct)], writes=["cin%d" % s])
                        for kk in range(4):
                            S.emit("pool", lambda e, s=s, kk=kk, ct=ct: e.tensor_scalar(out=dg[s][:, kk, :], in0=ident_f[:], scalar1=cw[:, ct * 4 + kk:ct * 4 + kk + 1],
                                                                                     scalar2=None, op0=ALU.mult),
                                   reads=["ident_f", "cw"], writes=["dg%d" % s])
                        for n in range(NCH):
                            ps = pi % 4
                            pi += 1
                            for kk in range(4):
                                S.emit("pe", lambda e, s=s, kk=kk, n=n, ps=ps: e.matmul(pc[ps][:], lhsT=dg[s][:, kk, :], rhs=cin[s][:, n * 512 + kk:n * 512 + kk + 512],
                                                                                     start=(kk == 0), stop=(kk == 3)),
                                       reads=["dg%d" % s, "cin%d" % s], writes=["pc%d" % ps])
                            S.emit("act", lambda e, ct=ct, n=n, ps=ps: e.activation(out=xc[:, ct, n * 512:(n + 1) * 512], in_=pc[ps][:], func=AF.Silu, bias=cb[:, ct:ct + 1]),
                                   reads=["pc%d" % ps, "cb"], writes=[("xc", ct)])
                    S.barrier()
                    S.flush()

                if "xc_s" in debug:
                    xc_s = dscr("xc_s", [1280, L], BF16)
                    dma("sp", xc_s.rearrange("(c p) t -> p c t", p=128), xc[:], reads=[("xc", i) for i in range(10)], writes=["xc_s"])
                    S.barrier()
                    S.flush()
                with ExitStack() as ph2:
                    if upto < 3:
                        raise_skip = True
                    else:
                        raise_skip = False
                    Xdt = [sbt(ph2, "Xdt%d" % i, [128, D], BF16) for i in range(2)]
                    XD = [sbt(ph2, "XD%d" % i, [128, D], F32) for i in range(2)]
                    Xdec = [sbt(ph2, "Xdec%d" % i, [128, D], BF16) for i in range(2)]
                    Btok = [sbt(ph2, "Btok%d" % i, [128, 128], BF16) for i in range(2)]
                    CBm = [sbt(ph2, "CBm%d" % i, [128, 2, 128], F32) for i in range(2)]
                    exps = [sbt(ph2, "exps%d" % i, [128, 48], F32) for i in range(2)]
                    rhsD = [sbt(ph2, "rhsD%d" % i, [128, 16, 128], F32) for i in range(2)]
                    Lm = [sbt(ph2, "Lm%d" % i, [128, 512], F32) for i in range(2)]
                    MT = [sbt(ph2, "MT%d" % i, [128, 16, 128], BF16) for i in range(2)]
                    Hs = sbt(ph2, "Hs", [128, 512], F32)
                    Hbf = sbt(ph2, "Hbf", [128, 512], BF16)
                    yoffs = sbt(ph2, "yoffs", [128, D], F32)
                    ysb = [sbt(ph2, "ysb%d" % i, [128, D], F32) for i in range(2)]
                    zt = [sbt(ph2, "zt%d" % i, [128, D], BF16) for i in range(2)]
                    yn = [sbt(ph2, "yn%d" % i, [128, D], BF16) for i in range(2)]
                    junk2 = sbt(ph2, "junk2", [128, 512], BF16)
                    ss2 = [sbt(ph2, "ss2_%d" % i, [128, 2], F32) for i in range(2)]
                    rs2 = [sbt(ph2, "rs2_%d" % i, [128, 2], F32) for i in range(2)]
                    stgT = [sbt(ph2, "stgT%d" % i, [128, KT, 512], BF16) for i in range(2)]
                    psm = pst(ph2, "psm", [128, 512], F32)
                    pX = pst(ph2, "pX", [128, KT, 128], BF16)
                    pBt_full = pst(ph2, "pBt", [128, 1024], BF16)
                    pBt = pBt_full[:, 0:128]
                    pD = [pst(ph2, "pD%d" % i, [128, 512], F32) for i in range(2)]
                    pY = [pst(ph2, "pY%d" % i, [128, 512], F32) for i in range(2)]
                    pO = pst(ph2, "pO", [128, 512], F32)
                    Cz = sbt(ph2, "Cz", [128, 2, L], BF16)
                    S.emit("pool", lambda e: e.memset(Cz[:], 0.0), writes=["Cz"])
                    S.emit("act", lambda e: e.copy(out=Cz[0:64, 0, :], in_=xc[0:64, 9, :]), reads=[("xc", 9), "Cz"], writes=["Cz"])
                    S.emit("dve", lambda e: e.tensor_copy(out=Cz[64:128, 1, :], in_=xc[64:128, 9, :]), reads=[("xc", 9), "Cz"], writes=["Cz"])
                    S.emit("pool", lambda e: e.memset(Hs[:], 0.0), writes=["Hs"])
                    S.emit("pool", lambda e: e.memset(Hbf[:], 0.0), writes=["Hbf"])
                    def stage1a(c):
                        s = c % 2
                        cols = slice(c * 128, (c + 1) * 128)
                        K_ = lambda n, s=s: "%s%d" % (n, s)
                        S.emit("pe", lambda e, c=c: e.matmul(psm[:, 0:16], lhsT=T32[:], rhs=dA[:, c, :], start=True, stop=True), reads=["T32", "dA"], writes=["psmA"])
                        S.emit("pe", lambda e, c=c: e.matmul(psm[:, 16:32], lhsT=U32[:], rhs=dA[:, c, :], start=True, stop=True), reads=["U32", "dA"], writes=["psmA"])
                        S.emit("pe", lambda e, c=c: e.matmul(psm[:, 32:48], lhsT=ones32[:], rhs=dA[:, c, :], start=True, stop=True), reads=["ones32", "dA"], writes=["psmA"])
                        S.emit("act", lambda e, s=s: e.activation(out=exps[s][:], in_=psm[:, 0:48], func=AF.Exp), reads=["psmA"], writes=[K_("exps")])
                        S.emit("pool", lambda e, s=s, c=c: e.tensor_tensor(out=rhsD[s][:], in0=T32[:].unsqueeze(1).to_broadcast([128, 16, 128]),
                                                                          in1=dA[:, c, :].unsqueeze(2).to_broadcast([128, 16, 128]), op=ALU.mult),
                               reads=["T32", "dA"], writes=[K_("rhsD")])
                        S.mark(1)
                        for k in range(KT):
                            S.emit("pe", lambda e, k=k, cols=cols: e.transpose(pX[:, k, :], xc[:, k, cols], ident_b[:]), reads=[("xc", k), "ident_b"], writes=["pX"])
                        S.emit("pe", lambda e, cols=cols: e.transpose(pBt, xc[:, 8, cols], ident_b[:]), reads=[("xc", 8), "ident_b"], writes=["pBt"])
                        S.emit("dve", lambda e, s=s, c=c: e.tensor_tensor(out=Xdt[s][:].rearrange("p (h d) -> p h d", h=16), in0=pX[:].rearrange("p k (a d) -> p (k a) d", a=2),
                                                                         in1=dtv[:, c, :].unsqueeze(2).to_broadcast([128, 16, 64]), op=ALU.mult),
                               reads=["pX", "dtv"], writes=[K_("Xdt")])
                        S.emit("dve", lambda e, s=s: e.tensor_tensor(out=XD[s][:].rearrange("p (h d) -> p h d", h=16), in0=pX[:].rearrange("p k (a d) -> p (k a) d", a=2),
                                                                    in1=dsk_bc[:].unsqueeze(2).to_broadcast([128, 16, 64]), op=ALU.mult),
                               reads=["pX", "dsk_bc"], writes=[K_("XD")])
                        S.emit("act", lambda e, s=s: e.copy(out=Btok[s][:], in_=pBt), reads=["pBt"], writes=[K_("Btok")])
                        S.mark(2)
                        for g in range(2):
                            S.emit("pe", lambda e, g=g, cols=cols: e.matmul(psm[:, 64 + g * 128:64 + (g + 1) * 128], lhsT=xc[:, 8, cols],
                                                                          rhs=Cz[:, g, cols], start=True, stop=True),
                                   reads=[("xc", 8), "Cz"], writes=["psmCB"])
                        S.emit("dve", lambda e, s=s: e.tensor_tensor(out=CBm[s][:], in0=psm[:, 64:320].rearrange("p (g l) -> p g l", g=2),
                                                                    in1=T32[:].unsqueeze(1).to_broadcast([128, 2, 128]), op=ALU.mult),
                               reads=["psmCB", "T32"], writes=[K_("CBm")])
                        S.mark(3)
                        for j in range(4):
                            jj = j % 2
                            S.emit("pe", lambda e, s=s, j=j, jj=jj: e.matmul(pD[jj][:], lhsT=U32[:], rhs=rhsD[s][:, 4 * j:4 * j + 4, :].rearrange("p h l -> p (h l)"),
                                                                            start=True, stop=True), reads=["U32", K_("rhsD")], writes=["pD%d" % jj])
                            S.emit("act", lambda e, jj=jj: e.activation(out=Lm[jj][:], in_=pD[jj][:], func=AF.Exp), reads=["pD%d" % jj], writes=["Lm%d" % jj])
                            S.emit("dve", lambda e, s=s, j=j, jj=jj: e.tensor_tensor(out=MT[s][:, 4 * j:4 * j + 4, :], in0=Lm[jj][:].rearrange("p (h l) -> p h l", h=4),
                                                                                    in1=CBm[s][:, j // 2, :].unsqueeze(1).to_broadcast([128, 4, 128]), op=ALU.mult),
                                   reads=["Lm%d" % jj, K_("CBm")], writes=[K_("MT")])
                        S.mark(4)
                        S.emit("dve", lambda e, s=s: e.tensor_tensor(out=Xdec[s][:].rearrange("p (h d) -> p h d", h=16), in0=Xdt[s][:].rearrange("p (h d) -> p h d", h=16),
                                                                    in1=exps[s][:, 16:32].unsqueeze(2).to_broadcast([128, 16, 64]), op=ALU.mult),
                               reads=[K_("Xdt"), K_("exps")], writes=[K_("Xdec")])

                    def stage1b(c):
                        s = c % 2
                        cols = slice(c * 128, (c + 1) * 128)
                        K_ = lambda n, s=s: "%s%d" % (n, s)
                        for h in range(16):
                            S.emit("pe", lambda e, s=s, h=h: e.matmul(pY[h // 8][:, (h % 8) * 64:(h % 8 + 1) * 64], lhsT=MT[s][:, h, :], rhs=Xdt[s][:, h * 64:(h + 1) * 64],
                                                                     start=True, stop=True), reads=[K_("MT"), K_("Xdt")], writes=["pY%d" % (h // 8)])
                        S.mark(5)
                        for g in range(2):
                            S.emit("pe", lambda e, g=g, cols=cols: e.matmul(pO[:], lhsT=Cz[:, g, cols], rhs=Hbf[:], start=True, stop=True),
                                   reads=["Cz", "Hbf"], writes=["pO"])
                            S.emit("dve", lambda e, g=g, s=s: e.tensor_tensor(out=yoffs[:, g * 512:(g + 1) * 512].rearrange("p (h d) -> p h d", h=8),
                                                                             in0=pO[:].rearrange("p (h d) -> p h d", h=8),
                                                                             in1=exps[s][:, g * 8:(g + 1) * 8].unsqueeze(2).to_broadcast([128, 8, 64]), op=ALU.mult),
                                   reads=["pO", K_("exps")], writes=["yoffs"])
                        S.mark(6)
                        for g in range(2):
                            S.emit("pe", lambda e, g=g, s=s: e.matmul(pD[g][:], lhsT=Btok[s][:], rhs=Xdec[s][:, g * 512:(g + 1) * 512], start=True, stop=True),
                                   reads=[K_("Btok"), K_("Xdec")], writes=["pD%d" % g])
                        for g in range(2):
                            rows = slice(g * 64, (g + 1) * 64)
                            S.emit("dve", lambda e, g=g, s=s, rows=rows: e.tensor_tensor(out=Hs[rows, :].rearrange("p (h d) -> p h d", h=8), in0=Hs[rows, :].rearrange("p (h d) -> p h d", h=8),
                                                                                       in1=exps[s][rows, 32 + g * 8:32 + (g + 1) * 8].unsqueeze(2).to_broadcast([64, 8, 64]), op=ALU.mult),
                                   reads=["Hs", K_("exps")], writes=["Hs"])
                            S.emit("dve", lambda e, g=g, rows=rows: e.tensor_tensor(out=Hs[rows, :], in0=pD[g][rows, :], in1=Hs[rows, :], op=ALU.add),
                                   reads=["Hs", "pD%d" % g], writes=["Hs"])
                            S.emit("act", lambda e, rows=rows: e.copy(out=Hbf[rows, :], in_=Hs[rows, :]), reads=["Hs"], writes=["Hbf"])
                        S.mark(7)

                        for b in range(2):
                            S.emit("dve", lambda e, b=b, s=s: e.tensor_tensor(out=ysb[s][:, b * 512:(b + 1) * 512], in0=pY[b][:], in1=yoffs[:, b * 512:(b + 1) * 512], op=ALU.add),
                                   reads=["pY%d" % b, "yoffs"], writes=[K_("ysb")])
                        S.emit("pool", lambda e, s=s: e.tensor_tensor(out=ysb[s][:], in0=ysb[s][:], in1=XD[s][:], op=ALU.add), reads=[K_("ysb"), K_("XD")], writes=[K_("ysb")])

                    def stage2a(c):
                        s = c % 2
                        K_ = lambda n, s=s: "%s%d" % (n, s)
                        dma("sp", zt[s][:], zs_s[c * 128:(c + 1) * 128, :], reads=[("zs", c)], writes=[K_("zt")])
                        S.emit("pool", lambda e, s=s: e.tensor_tensor(out=ysb[s][:], in0=ysb[s][:], in1=zt[s][:], op=ALU.mult), reads=[K_("ysb"), K_("zt")], writes=[K_("ysb")])
                        for g in range(2):
                            S.emit("act", lambda e, g=g, s=s: e.activation(out=junk2[:], in_=ysb[s][:, g * 512:(g + 1) * 512], func=AF.Square, accum_out=ss2[s][:, g:g + 1]),
                                   reads=[K_("ysb")], writes=["junk2", K_("ss2_")])
                        S.emit("act", lambda e, s=s: e.activation(out=rs2[s][:], in_=ss2[s][:], func=AF.Ln, scale=1.0 / 512, bias=EPS_T[:, 0:1]),
                               reads=[K_("ss2_"), "eps_t"], writes=[K_("rs2_")])
                        S.emit("act", lambda e, s=s: e.activation(out=rs2[s][:], in_=rs2[s][:], func=AF.Exp, scale=-0.5), reads=[K_("rs2_")], writes=[K_("rs2_")])
                        for g in range(2):
                            S.emit("dve", lambda e, g=g, s=s: e.scalar_tensor_tensor(out=yn[s][:, g * 512:(g + 1) * 512], in0=ysb[s][:, g * 512:(g + 1) * 512], scalar=rs2[s][:, g:g + 1],
                                                                                    in1=snwbc[:, g * 512:(g + 1) * 512], op0=ALU.mult, op1=ALU.mult),
                                   reads=[K_("ysb"), K_("rs2_"), "snwbc"], writes=[K_("yn")])

                    def stage2b(c):
                        s = c % 2
                        K_ = lambda n, s=s: "%s%d" % (n, s)
                        for k in range(KT):
                            S.emit("pe", lambda e, k=k, s=s: e.transpose(pX[:, k, :], yn[s][:, k * 128:(k + 1) * 128], ident_b[:]), reads=[K_("yn"), "ident_b"], writes=["pX"])
                        sgi = (c // 4) % 2
                        S.emit("act", lambda e, sgi=sgi, c=c: e.copy(out=stgT[sgi][:, :, (c % 4) * 128:(c % 4 + 1) * 128], in_=pX[:]), reads=["pX"], writes=["stgT%d" % sgi])
                        if c % 4 == 3:
                            n = c // 4
                            for k in range(KT):
                                dma("sp", ssdT_s[k * 128:(k + 1) * 128, n * 512:(n + 1) * 512], stgT[sgi][:, k, :], reads=["stgT%d" % sgi], writes=[("ssdT", n, k)])

                    NCk = 0 if raise_skip else NT
                    for i in range(NCk + 2):
                        if 2 <= i:
                            stage2a(i - 2)
                        if i < NCk:
                            stage1a(i)
                        if 1 <= i <= NCk:
                            stage1b(i - 1)
                        if 2 <= i:
                            stage2b(i - 2)
                    S.barrier()
                    S.flush()

        if upto >= 4:
            with ExitStack() as ph:
                vall = sbt(ph, "vall", [128, NT, 1040], BF16)
                gA = sbt(ph, "gA", [128, NT * 32], BF16)
                gM = sbt(ph, "gM", [128, NT * 32], BF16)
                gO = sbt(ph, "gO", [128, NT * 32], BF16)
                Sel = sbt(ph, "Sel", [128, 64], BF16)
                RD = [sbt(ph, "RD%d" % i, [65, 512], BF16) for i in range(2)]
                numS = [sbt(ph, "numS%d" % i, [64, 512], F32) for i in range(2)]
                attTh = [sbt(ph, "attTh%d" % i, [64, L], BF16) for i in range(2)]
                cmk = sbt(ph, "cmk", [128, 2048], BF16)
                qsb = [sbt(ph, "qsb_%d" % i, [128, L], BF16) for i in range(2)]
                ksb = [sbt(ph, "ksb_%d" % i, [128, L], BF16) for i in range(2)]
                kz = sbt(ph, "kz", [128, 2, L], BF16)
                qz = sbt(ph, "qz", [128, 2, L], BF16)
                negmw = sbt(ph, "negmw", [128, NT, 96], BF16)
                kms = sbt(ph, "kms", [128, 16], F32)
                kmz = sbt(ph, "kmz", [128, 2, 16], BF16)
                gate = sbt(ph, "gate", [128, NT, 2, 16], F32)
                top8 = sbt(ph, "top8", [128, NT, 2, 8], F32)
                alw = sbt(ph, "alw", [128, NT, 2, 16], F32)
                pTs = [sbt(ph, "pTs%d" % i, [128, 512], BF16) for i in range(8)]
                pS = [pst(ph, "pS%d" % i, [128, 512], F32) for i in range(6)]
                pOt = [pst(ph, "pOt%d" % i, [128, 512], F32) for i in range(2)]
                pG = pS[0]
                pBC = pS[1]
                dma("pool", gA[:], c_gA_d.partition_broadcast(128), writes=["gA"])
                dma("pool", gM[:], c_gM_d.partition_broadcast(128), writes=["gM"])
                dma("pool", gO[:], c_gO_d.partition_broadcast(128), writes=["gO"])
                S.emit("pool", lambda e: e.memset(Sel[:], 0.0), writes=["Sel"])
                S.emit("pool", lambda e: e.memset(Sel[64:65, :], 1.0), reads=["Sel"], writes=["Sel"])
                for i in range(2):
                    S.emit("pool", lambda e, i=i: e.memset(RD[i][:], 0.0), writes=["RD%d" % i])
                dma("pool", cmk[:], c_cm_d, writes=["cmk"])
                S.emit("pool", lambda e: e.memset(kz[:], 0.0), writes=[("kz", 0), ("kz", 1)])
                S.emit("pool", lambda e: e.memset(qz[:], 0.0), writes=[("qz", 0), ("qz", 1)])
                S.emit("pool", lambda e: e.memset(negmw[:], 0.0), writes=["negmw"])
                dma("pool", kz[64:80, 0, :], c_blkoh_d[:, 0:L], reads=[("kz", 0)], writes=[("kz", 0)])
                dma("pool", kz[0:16, 1, :], c_blkoh_d[:, 0:L], reads=[("kz", 1)], writes=[("kz", 1)])
                S.emit("pool", lambda e: e.memset(kmz[:], 0.0), writes=["kmz"])
                S.emit("pool", lambda e: e.memset(kms[:], 0.0), writes=["kms"])
                psn = [0]
                pt_i = 0
                po_i = 0
                for hp in range(8):
                    s = hp % 2
                    dma("sp", qsb[s][:], qT_s[hp * 128:(hp + 1) * 128, :], reads=[("qT", hp)], writes=["qsb_%d" % s])
                    dma("sp", ksb[s][:], kT_s[hp * 128:(hp + 1) * 128, :], reads=[("kT", hp)], writes=["ksb_%d" % s])
                    dma("sp", kz[0:64, 0, :], kT_s[hp * 128:hp * 128 + 64, :], reads=[("kT", hp), ("kz", 0)], writes=[("kz", 0)])
                    dma("sp", kz[64:128, 1, :], kT_s[hp * 128 + 64:(hp + 1) * 128, :], reads=[("kT", hp), ("kz", 1)], writes=[("kz", 1)])
                    dma("sp", qz[0:64, 0, :], qT_s[hp * 128:hp * 128 + 64, :], reads=[("qT", hp), ("qz", 0)], writes=[("qz", 0)])
                    dma("sp", qz[64:128, 1, :], qT_s[hp * 128 + 64:(hp + 1) * 128, :], reads=[("qT", hp), ("qz", 1)], writes=[("qz", 1)])
                    if hp == 0:
                        for t in range(NT):
                            dma("sp", vall[:, t, :], vaug_s[t * 128:(t + 1) * 128, :], reads=[("vaug", t)], writes=[("vall", t)])
                    S.emit("dve", lambda e, s=s: e.tensor_reduce(out=kms[:, 0:NB], in_=ksb[s][:].rearrange("p (n k) -> p n k", k=256), axis=AX.X, op=ALU.add),
                           reads=["ksb_%d" % s], writes=["kms"])
                    S.emit("dve", lambda e: e.tensor_scalar(out=kmz[0:64, 0, :], in0=kms[0:64, :], scalar1=1.0 / 256, scalar2=None, op0=ALU.mult), reads=["kms"], writes=["kmz"])
                    S.emit("dve", lambda e: e.tensor_scalar(out=kmz[64:128, 1, :], in0=kms[64:128, :], scalar1=1.0 / 256, scalar2=None, op0=ALU.mult), reads=["kms"], writes=["kmz"])
                    for t0 in range(0, NT, 16):
                        nt = min(16, NT - t0)
                        for t in range(t0, t0 + nt):
                            S.emit("pe", lambda e, s=s, t=t, t0=t0: e.matmul(pG[:, (t - t0) * 32:(t - t0 + 1) * 32], lhsT=qsb[s][:, t * 128:(t + 1) * 128],
                                                                            rhs=kmz[:].rearrange("p a n -> p (a n)"), start=True, stop=True),
                                   reads=["qsb_%d" % s, "kmz"], writes=["pS0"])
                        S.emit("dve", lambda e, t0=t0, nt=nt: e.tensor_copy(out=gate[:, t0:t0 + nt, :, :].rearrange("p t a n -> p (t a n)"), in_=pG[:, 0:nt * 32]),
                               reads=["pS0"], writes=["gate"])
                    S.emit("dve", lambda e: e.tensor_tensor(out=gate[:].rearrange("p t a n -> p (t a n)"), in0=gate[:].rearrange("p t a n -> p (t a n)"), in1=gA[:], op=ALU.add),
                           reads=["gate", "gA"], writes=["gate"])
                    for t in range(NT):
                        for a in range(2):
                            S.emit("dve", lambda e, t=t, a=a: e.max(out=top8[:, t, a, :], in_=gate[:, t, a, :]), reads=["gate"], writes=["top8"])
                    S.emit("dve", lambda e: e.tensor_tensor(out=alw[:].rearrange("p t a n -> p (t a) n"), in0=gate[:].rearrange("p t a n -> p (t a) n"),
                                                            in1=top8[:].rearrange("p t a k -> p (t a) k")[:, :, 2:3].to_broadcast([128, NT * 2, 16]), op=ALU.is_ge),
                           reads=["gate", "top8"], writes=["alw"])
                    S.emit("dve", lambda e: e.tensor_tensor(out=alw[:].rearrange("p t a n -> p (t a n)"), in0=alw[:].rearrange("p t a n -> p (t a n)"), in1=gM[:], op=ALU.mult),
                           reads=["alw", "gM"], writes=["alw"])
                    S.emit("dve", lambda e: e.tensor_tensor(out=alw[:].rearrange("p t a n -> p (t a n)"), in0=alw[:].rearrange("p t a n -> p (t a n)"), in1=gO[:], op=ALU.add),
                           reads=["alw", "gO"], writes=["alw"])
                    S.emit("dve", lambda e: e.tensor_scalar(out=negmw[:, :, 64:96], in0=alw[:].rearrange("p t a n -> p t (a n)"),
                                                            scalar1=-1.0, scalar2=BIG, op0=ALU.add, op1=ALU.mult), reads=["alw"], writes=["negmw"])
                    for a in range(2):
                        for t0 in range(0, NT, 4):
                            pgj = (t0 // 4) % 6
                            pgt, pgk = pS[pgj], "pS%d" % pgj
                            for t in range(t0, t0 + 4):
                                if a == 0:
                                    S.emit("pe", lambda e, t=t, t0=t0, pgt=pgt: e.matmul(pgt[0:80, (t - t0) * 128:(t - t0 + 1) * 128], lhsT=negmw[:, t, 0:80], rhs=ident_b[:], start=True, stop=True),
                                           reads=["negmw", "ident_b"], writes=[pgk])
                                else:
                                    S.emit("pe", lambda e, t=t, t0=t0, pgt=pgt: e.matmul(pgt[0:16, (t - t0) * 128:(t - t0 + 1) * 128], lhsT=negmw[:, t, 80:96], rhs=ident_b[:], start=True, stop=True),
                                           reads=["negmw", "ident_b"], writes=[pgk])
                            if a == 0:
                                S.emit("act", lambda e, t0=t0, pgt=pgt: e.copy(out=qz[64:80, 0, t0 * 128:(t0 + 4) * 128], in_=pgt[64:80, :]), reads=[pgk, ("qz", 0)], writes=[("qz", 0)])
                            else:
                                S.emit("dve", lambda e, t0=t0, pgt=pgt: e.tensor_copy(out=qz[0:16, 1, t0 * 128:(t0 + 4) * 128], in_=pgt[0:16, :]), reads=[pgk, ("qz", 1)], writes=[("qz", 1)])
                    for a in range(2):
                        h = hp * 2 + a
                        ah = h % 2
                        items = [(qc, kt) for qc in range(NCH) for kt in range(4 * (qc + 1))]

                        def emit_scores(qc, kt, slot, a=a):
                            r = kt - 4 * qc
                            c0 = max(r, 0) * 128
                            qcols = slice(qc * 512 + c0, (qc + 1) * 512)
                            S.emit("pe", lambda e: e.matmul(pS[slot][:, c0:512], lhsT=kz[:, a, kt * 128:(kt + 1) * 128], rhs=qz[:, a, qcols], start=True, stop=(r < 0)),
                                   reads=[("kz", a), ("qz", a)], writes=["pS%d" % slot])
                            if r >= 0:
                                S.emit("pe", lambda e: e.matmul(pS[slot][:, c0:512], lhsT=ident_b[:], rhs=cmk[:, r * 512 + c0:(r + 1) * 512], start=False, stop=True),
                                       reads=["ident_b", "cmk"], writes=["pS%d" % slot])

                        slots = {}
                        pending = []
                        AHEAD = 4
                        for ii in range(min(AHEAD, len(items))):
                            slots[ii] = psn[0] % 6
                            psn[0] += 1
                            emit_scores(items[ii][0], items[ii][1], slots[ii])
                        for ii, (qc, kt) in enumerate(items):
                            if ii + AHEAD < len(items):
                                slots[ii + AHEAD] = psn[0] % 6
                                psn[0] += 1
                                emit_scores(items[ii + AHEAD][0], items[ii + AHEAD][1], slots[ii + AHEAD])
                            slot = slots[ii]
                            pt = pt_i % 8
                            pt_i += 1
                            r = kt - 4 * qc
                            c0 = max(r, 0) * 128
                            S.emit("act", lambda e, slot=slot, pt=pt, c0=c0: e.activation(out=pTs[pt][:, c0:512], in_=pS[slot][:, c0:512], func=AF.Exp, scale=0.125),
                                   reads=["pS%d" % slot], writes=["pTs%d" % pt])
                            if kt == 0:
                                po = po_i % 2
                                po_i += 1
                            S.emit("pe", lambda e, pt=pt, po=po, kt=kt, h=h, qc=qc, c0=c0: e.matmul(
                                pOt[po][0:65, c0:512], lhsT=vall[:, kt, h * 65:(h + 1) * 65], rhs=pTs[pt][:, c0:512],
                                start=(kt == 0), stop=(kt == 4 * qc + 3)),
                                reads=["pTs%d" % pt, ("vall", kt)], writes=["pOt%d" % po])
                            def norm_tail(po=po, qc=qc, ah=ah):
                                bs = psn[0] % 6
                                psn[0] += 1
                                S.emit("pe", lambda e: e.matmul(pS[bs][0:64, :], lhsT=Sel[0:65, :], rhs=RD[po][:], start=True, stop=True),
                                       reads=["Sel", "RD%d" % po], writes=["pS%d" % bs])
                                S.emit("dve", lambda e: e.tensor_tensor(out=attTh[ah][:, qc * 512:(qc + 1) * 512], in0=pS[bs][0:64, :], in1=numS[po][:], op=ALU.mult),
                                       reads=["pS%d" % bs, "numS%d" % po], writes=["attTh%d" % ah])
                            for pn in list(pending):
                                pn[0] -= 1
                                if pn[0] <= 0:
                                    pn[1]()
                                    pending.remove(pn)
                            if kt == 4 * qc + 3:
                                S.emit("dve", lambda e, po=po: e.reciprocal(out=RD[po][64:65, :], in_=pOt[po][64:65, :]), reads=["pOt%d" % po], writes=["RD%d" % po])
                                S.emit("act", lambda e, po=po: e.copy(out=numS[po][:], in_=pOt[po][0:64, :]), reads=["pOt%d" % po], writes=["numS%d" % po])
                                pending.append([7, norm_tail])
                        for pn in pending:
                            pn[1]()
                        pending = []
                        dma("sp", attT_s[h * 64:(h + 1) * 64, :], attTh[ah][:], reads=["attTh%d" % ah], writes=[("attT", h)])
                S.barrier()
                S.flush()

        if upto >= 5:
            h2T_s = dscr("h2T_s", [D, L], BF16)
            with ExitStack() as ph:
                Wso = sbt(ph, "Wso", [128, KT, D], BF16)
                Wao = sbt(ph, "Wao", [128, KT, D], BF16)
                Wo = sbt(ph, "Wo", [128, KT, D], BF16)
                nw2bc = sbt(ph, "nw2bc", [128, D], F32)
                ssc = [sbt(ph, "ssc%d" % i, [128, KT, 512], BF16) for i in range(2)]
                atc = [sbt(ph, "atc%d" % i, [128, KT, 512], BF16) for i in range(2)]
                sgs = [sbt(ph, "sgs%d" % i, [128, KT, 512], BF16) for i in range(2)]
                sga = [sbt(ph, "sga%d" % i, [128, KT, 512], BF16) for i in range(2)]
                mT = [sbt(ph, "mT%d" % i, [128, KT, 512], BF16) for i in range(2)]
                m1 = [sbt(ph, "m1_%d" % i, [128, 512], F32) for i in range(2)]
                m2 = [sbt(ph, "m2_%d" % i, [128, 512], F32) for i in range(2)]
                xin2 = [sbt(ph, "xin2_%d" % i, [128, D], F32) for i in range(2)]
                x1t = [sbt(ph, "x1t%d" % i, [128, D], F32) for i in range(2)]
                hn2 = [sbt(ph, "hn2_%d" % i, [128, D], BF16) for i in range(2)]
                junk3 = sbt(ph, "junk3", [128, D], BF16)
                ssd_ = [sbt(ph, "ssD%d" % i, [128, 1], F32) for i in range(2)]
                rsd_ = [sbt(ph, "rsD%d" % i, [128, 1], F32) for i in range(2)]
                h2st = [sbt(ph, "h2st%d" % i, [128, KT, 512], BF16) for i in range(2)]
                pA = [pst(ph, "pA%d" % i, [128, 512], F32) for i in range(2)]
                pB = [pst(ph, "pB%d" % i, [128, 512], F32) for i in range(2)]
                pX1 = [pst(ph, "pX1_%d" % i, [128, 512], F32) for i in range(2)]
                pTd = pst(ph, "pTd", [128, KT, 128], BF16)
                for cb in range(0, KT, 2):
                    csl = slice(cb * 128, (cb + 2) * 128)
                    dma("pool", Wso[:, :, csl], wso_d[:, csl].rearrange("(k p) c -> p k c", p=128), writes=[("Wso", cb), ("Wso", cb + 1)])
                    dma("pool", Wao[:, :, csl], wao_d[:, csl].rearrange("(k p) c -> p k c", p=128), writes=[("Wao", cb), ("Wao", cb + 1)])
                for hb in range(2):
                    hsl = slice(hb * 512, (hb + 1) * 512)
                    dma("pool", Wo[:, :, hsl], wo_d[:, hsl].rearrange("(k p) c -> p k c", p=128), writes=[("Wo", hb)])
                dma("sp", nw2bc[:], nw2_d.partition_broadcast(128), writes=["nw2bc"])
                ab_i = 0
                x_i = 0
                deferredD = []
                for n in range(NCH):
                    s = n % 2
                    cs = slice(n * 512, (n + 1) * 512)
                    dma("sp", ssc[s][:], ssdT_s[:, cs].rearrange("(k p) t -> p k t", p=128), writes=["ssc%d" % s])
                    dma("sp", atc[s][:], attT_s[:, cs].rearrange("(k p) t -> p k t", p=128), writes=["atc%d" % s])
                    dma("sp", sgs[s][:], sgT_s[0:D, cs].rearrange("(k p) t -> p k t", p=128), writes=["sgs%d" % s])
                    dma("sp", sga[s][:], sgT_s[D:2 * D, cs].rearrange("(k p) t -> p k t", p=128), writes=["sga%d" % s])
                    for c in range(KT):
                        i = ab_i % 2
                        ab_i += 1
                        for k in range(KT):
                            S.emit("pe", lambda e, i=i, k=k, c=c, s=s: e.matmul(pA[i][:], lhsT=Wso[:, k, c * 128:(c + 1) * 128], rhs=ssc[s][:, k, :], start=(k == 0), stop=(k == KT - 1)),
                                   reads=[("Wso", c), "ssc%d" % s], writes=["pA%d" % i])
                        for k in range(KT):
                            S.emit("pe", lambda e, i=i, k=k, c=c, s=s: e.matmul(pB[i][:], lhsT=Wao[:, k, c * 128:(c + 1) * 128], rhs=atc[s][:, k, :], start=(k == 0), stop=(k == KT - 1)),
                                   reads=[("Wao", c), "atc%d" % s], writes=["pB%d" % i])
                        S.emit("dve", lambda e, i=i, c=c, s=s: e.tensor_tensor(out=m1[i][:], in0=pA[i][:], in1=sgs[s][:, c, :], op=ALU.mult),
                               reads=["pA%d" % i, "sgs%d" % s], writes=["m1_%d" % i])
                        S.emit("dve", lambda e, i=i, c=c, s=s: e.tensor_tensor(out=m2[i][:], in0=pB[i][:], in1=sga[s][:, c, :], op=ALU.mult),
                               reads=["pB%d" % i, "sga%d" % s], writes=["m2_%d" % i])
                        S.emit("pool", lambda e, i=i, c=c, s=s: e.tensor_tensor(out=mT[s][:, c, :], in0=m1[i][:], in1=m2[i][:], op=ALU.add),
                               reads=["m1_%d" % i, "m2_%d" % i], writes=["mT%d" % s])
                    def d_s1(n, j, s):
                        t = n * 4 + j
                        xs = t % 2
                        dma("sp", xin2[xs][:], x_d[t * 128:(t + 1) * 128, :], writes=["xin2_%d" % xs])
                        for hf in range(2):
                            for k in range(KT):
                                S.emit("pe", lambda e, hf=hf, k=k, j=j, s=s: e.matmul(pX1[hf][:], lhsT=mT[s][:, k, j * 128:(j + 1) * 128], rhs=Wo[:, k, hf * 512:(hf + 1) * 512],
                                                                                     start=(k == 0), stop=(k == KT - 1)),
                                       reads=[("Wo", hf), "mT%d" % s], writes=["pX1_%d" % hf])
                            S.emit("dve", lambda e, hf=hf, xs=xs: e.tensor_tensor(out=x1t[xs][:, hf * 512:(hf + 1) * 512], in0=pX1[hf][:], in1=xin2[xs][:, hf * 512:(hf + 1) * 512], op=ALU.add),
                                   reads=["pX1_%d" % hf, "xin2_%d" % xs], writes=["x1t%d" % xs])
                        dma("sp", x1_s[t * 128:(t + 1) * 128, :], x1t[xs][:], reads=["x1t%d" % xs], writes=[("x1s", t)])
                        S.emit("act", lambda e, xs=xs: e.activation(out=junk3[:], in_=x1t[xs][:], func=AF.Square, accum_out=ssd_[xs][:]),
                               reads=["x1t%d" % xs], writes=["junk3", "ssD%d" % xs])
                        rstd_from_ss(ssd_[xs][:], rsd_[xs][:], D, ["ssD%d" % xs], ["rsD%d" % xs])
                        S.emit("dve", lambda e, xs=xs: e.scalar_tensor_tensor(out=hn2[xs][:], in0=x1t[xs][:], scalar=rsd_[xs][:, 0:1], in1=nw2bc[:], op0=ALU.mult, op1=ALU.mult),
                               reads=["x1t%d" % xs, "rsD%d" % xs, "nw2bc"], writes=["hn2_%d" % xs])

                    def d_s2(n, j, s):
                        t = n * 4 + j
                        xs = t % 2
                        for k in range(KT):
                            S.emit("pe", lambda e, k=k, xs=xs: e.transpose(pTd[:, k, :], hn2[xs][:, k * 128:(k + 1) * 128], ident_b[:]), reads=["hn2_%d" % xs, "ident_b"], writes=["pTd"])
                        S.emit("act", lambda e, s=s, j=j: e.copy(out=h2st[s][:, :, j * 128:(j + 1) * 128], in_=pTd[:]), reads=["pTd"], writes=["h2st%d" % s])

                    def d_store(n, s):
                        cs_ = slice(n * 512, (n + 1) * 512)
                        for k in range(KT):
                            dma("sp", h2T_s[k * 128:(k + 1) * 128, cs_], h2st[s][:, k, :], reads=["h2st%d" % s], writes=[("h2T", n, k)])

                    for fn in deferredD:
                        fn()
                    deferredD = []
                    d_s1(n, 0, s)
                    d_s1(n, 1, s)
                    d_s2(n, 0, s)
                    d_s1(n, 2, s)
                    d_s2(n, 1, s)
                    d_s1(n, 3, s)
                    d_s2(n, 2, s)
                    deferredD = [lambda n=n, s=s: d_s2(n, 3, s), lambda n=n, s=s: d_store(n, s)]
                for fn in deferredD:
                    fn()
                S.barrier()
                S.flush()

        if upto >= 6:
            PTOK = min(L, 2048)
            for part in range(L // PTOK):
                tok0 = part * PTOK
                PT_ = PTOK // 128
                PC_ = PTOK // 512
                with ExitStack() as pp_:
                    acc = sbt(pp_, "acc", [128, PT_, D], F32)
                    Wpg = sbt(pp_, "Wpg", [128, KT, D], BF16)
                    Wple = sbt(pp_, "Wple", [128, 2, D], BF16)
                    nw3bc = sbt(pp_, "nw3bc", [128, D], F32)
                    nwfbc = sbt(pp_, "nwfbc", [128, D], F32)
                    with ExitStack() as ph:
                        h2 = sbt(ph, "h2", [128, KT, PTOK], BF16)
                        Wrt = sbt(ph, "Wrt", [128, KT, 36], BF16)
                        brt = sbt(ph, "brt", [128, 36], F32)
                        comb = sbt(ph, "comb", [128, PT_, 32], F32)
                        lgB = sbt(ph, "lgB", [128, 8, 36], F32)
                        gmxB = sbt(ph, "gmxB", [128, 8], F32)
                        gohB = sbt(ph, "gohB", [128, 8, 4], F32)
                        gexB = sbt(ph, "gexB", [128, 8, 4], F32)
                        gwB = sbt(ph, "gwB", [128, 8], F32)
                        eltB = sbt(ph, "eltB", [128, 8, 4, 8], F32)
                        elsB = sbt(ph, "elsB", [128, 8, 8], F32)
                        t8B = sbt(ph, "t8B", [128, 8, 8], F32)
                        scB = sbt(ph, "scB", [128, 7, 8], F32)
                        eaB = sbt(ph, "eaB", [128, 8, 8], F32)
                        ebB = sbt(ph, "ebB", [128, 8, 8], F32)
                        lg = sbt(ph, "lg", [128, 36], F32)
                        gmx = sbt(ph, "gmx", [128, 1], F32)
                        ngmx = sbt(ph, "ngmx", [128, 1], F32)
                        goh = sbt(ph, "goh", [128, 4], F32)
                        gex = sbt(ph, "gex", [128, 4], F32)
                        gsum = sbt(ph, "gsum", [128, 1], F32)
                        gw = sbt(ph, "gw", [128, 1], F32)
                        elt = sbt(ph, "elt", [128, 4, 8], F32)
                        els = sbt(ph, "els", [128, 8], F32)
                        t8 = sbt(ph, "t8", [128, 8], F32)
                        sc = sbt(ph, "sc", [128, 8], F32)
                        ea = sbt(ph, "ea", [128, 8], F32)
                        eb = sbt(ph, "eb", [128, 8], F32)
                        Wg = [sbt(ph, "Wg%d" % i, [128, KT, 256], BF16) for i in range(2)]
                        Wu = [sbt(ph, "Wu%d" % i, [128, KT, 256], BF16) for i in range(2)]
                        Wd = [sbt(ph, "Wd%d" % i, [128, 2, D], BF16) for i in range(2)]
                        hid = [sbt(ph, "hid%d" % i, [128, 2, PTOK], BF16) for i in range(2)]
                        sgt = [sbt(ph, "sgt%d" % i, [128, 512], F32) for i in range(2)]
                        etmp = [sbt(ph, "etmp%d" % i, [128, 512], F32) for i in range(2)]
                        pGt = [pst(ph, "pGt%d" % i, [128, 512], F32) for i in range(2)]
                        pUt = [pst(ph, "pUt%d" % i, [128, 512], F32) for i in range(2)]
                        pDn = [pst(ph, "pDn%d" % i, [128, 512], F32) for i in range(4)]
                        pR = pDn[3]
                        for k in range(KT):
                            dma("sp", h2[:, k, :], h2T_s[k * 128:(k + 1) * 128, tok0:tok0 + PTOK], writes=[("h2", k)])
                        dma("pool", Wrt[:], wrt_d.rearrange("(k p) c -> p k c", p=128), writes=["Wrt"])
                        dma("sp", brt[:], brt_d.partition_broadcast(128), writes=["brt"])
                        for t in range(PT_):
                            dma("sp", acc[:, t, :], x1_s[tok0 + t * 128:tok0 + (t + 1) * 128, :], reads=[("x1s", tok0 // 128 + t)], writes=[("acc", t)])
                        h2keys = [("h2", k) for k in range(KT)]
                        RB = 8
                        for t0 in range(0, PT_, RB):
                            TB = [128, RB, 8]
                            for tt in range(RB):
                                t = t0 + tt
                                for k in range(KT):
                                    S.emit("pe", lambda e, k=k, t=t, tt=tt: e.matmul(pR[:, tt * 36:(tt + 1) * 36], lhsT=h2[:, k, t * 128:(t + 1) * 128], rhs=Wrt[:, k, :],
                                                                                    start=(k == 0 and tt == 0), stop=(k == KT - 1)),
                                           reads=h2keys + ["Wrt"], writes=["pDn3"])
                            prv = pR[:, 0:RB * 36].rearrange("p (t c) -> p t c", t=RB)
                            S.emit("dve", lambda e, prv=prv: e.tensor_tensor(out=lgB[:], in0=prv, in1=brt[:].unsqueeze(1).to_broadcast([128, RB, 36]), op=ALU.add),
                                   reads=["pDn3", "brt"], writes=["lgB"])
                            S.emit("dve", lambda e: e.tensor_reduce(out=gmxB[:], in_=lgB[:, :, 0:4], axis=AX.X, op=ALU.max), reads=["lgB"], writes=["gmxB"])
                            S.emit("dve", lambda e: e.tensor_tensor(out=gohB[:], in0=lgB[:, :, 0:4], in1=gmxB[:].unsqueeze(2).to_broadcast([128, RB, 4]), op=ALU.is_ge),
                                   reads=["lgB", "gmxB"], writes=["gohB"])
                            S.emit("dve", lambda e: e.tensor_tensor(out=gexB[:], in0=lgB[:, :, 0:4], in1=gmxB[:].unsqueeze(2).to_broadcast([128, RB, 4]), op=ALU.subtract),
                                   reads=["lgB", "gmxB"], writes=["gexB"])
                            S.emit("act", lambda e: e.activation(out=gexB[:], in_=gexB[:], func=AF.Exp), reads=["gexB"], writes=["gexB"])
                            S.emit("dve", lambda e: e.tensor_reduce(out=gwB[:], in_=gexB[:], axis=AX.X, op=ALU.add), reads=["gexB"], writes=["gwB"])
                            S.emit("dve", lambda e: e.reciprocal(out=gwB[:], in_=gwB[:]), reads=["gwB"], writes=["gwB"])
                            S.emit("dve", lambda e: e.tensor_tensor(out=eltB[:], in0=lgB[:, :, 4:36].rearrange("p t (g e) -> p t g e", g=4),
                                                                    in1=gohB[:].unsqueeze(3).to_broadcast([128, RB, 4, 8]), op=ALU.mult),
                                   reads=["lgB", "gohB"], writes=["eltB"])
                            S.emit("dve", lambda e: e.tensor_reduce(out=elsB[:], in_=eltB[:].rearrange("p t g e -> p t e g"), axis=AX.X, op=ALU.add), reads=["eltB"], writes=["elsB"])
                            for tt in range(RB):
                                S.emit("dve", lambda e, tt=tt: e.max(out=t8B[:, tt, :], in_=elsB[:, tt, :]), reads=["elsB"], writes=["t8B"])
                            v1b = lambda: t8B[:, :, 0:1].to_broadcast(TB)
                            v2b = lambda: t8B[:, :, 1:2].to_broadcast(TB)
                            S.emit("dve", lambda e: e.tensor_tensor(out=scB[:, 0, :], in0=t8B[:, :, 1], in1=t8B[:, :, 0], op=ALU.subtract), reads=["t8B"], writes=["scB"])
                            S.emit("act", lambda e: e.activation(out=scB[:, 1, :], in_=scB[:, 0, :], func=AF.Exp), reads=["scB"], writes=["scB"])
                            S.emit("dve", lambda e: e.tensor_scalar(out=scB[:, 2, :], in0=scB[:, 1, :], scalar1=1.0, scalar2=None, op0=ALU.add), reads=["scB"], writes=["scB"])
                            S.emit("dve", lambda e: e.reciprocal(out=scB[:, 3, :], in_=scB[:, 2, :]), reads=["scB"], writes=["scB"])
                            S.emit("dve", lambda e: e.tensor_tensor(out=scB[:, 4, :], in0=scB[:, 3, :], in1=gwB[:], op=ALU.mult), reads=["scB", "gwB"], writes=["scB"])
                            S.emit("dve", lambda e: e.tensor_tensor(out=scB[:, 5, :], in0=gwB[:], in1=scB[:, 4, :], op=ALU.subtract), reads=["scB", "gwB"], writes=["scB"])
                            S.emit("dve", lambda e: e.tensor_tensor(out=scB[:, 6, :], in0=scB[:, 4, :], in1=scB[:, 5, :], op=ALU.subtract), reads=["scB"], writes=["scB"])
                            S.emit("dve", lambda e: e.tensor_tensor(out=eaB[:], in0=elsB[:], in1=v1b(), op=ALU.is_ge), reads=["elsB", "t8B"], writes=["eaB"])
                            S.emit("dve", lambda e: e.tensor_tensor(out=eaB[:], in0=eaB[:], in1=scB[:, 6, :].unsqueeze(2).to_broadcast(TB), op=ALU.mult), reads=["eaB", "scB"], writes=["eaB"])
                            S.emit("dve", lambda e: e.tensor_tensor(out=ebB[:], in0=elsB[:], in1=v2b(), op=ALU.is_ge), reads=["elsB", "t8B"], writes=["ebB"])
                            S.emit("dve", lambda e: e.tensor_tensor(out=ebB[:], in0=ebB[:], in1=scB[:, 5, :].unsqueeze(2).to_broadcast(TB), op=ALU.mult), reads=["ebB", "scB"], writes=["ebB"])
                            S.emit("dve", lambda e: e.tensor_tensor(out=eaB[:], in0=eaB[:], in1=ebB[:], op=ALU.add), reads=["eaB", "ebB"], writes=["eaB"])
                            for g in range(4):
                                S.emit("dve", lambda e, g=g, t0=t0: e.tensor_tensor(out=comb[:, t0:t0 + RB, g * 8:(g + 1) * 8], in0=eaB[:], in1=gohB[:, :, g:g + 1].to_broadcast(TB), op=ALU.mult),
                                       reads=["eaB", "gohB"], writes=["comb"])
                        cnt = {"gu": 0, "dn": 0}

                        def emit_gu(ex, n, f):
                            s = ex % 2
                            cs = slice(n * 512, (n + 1) * 512)
                            i = cnt["gu"] % 2
                            cnt["gu"] += 1
                            for k in range(KT):
                                S.emit("pe", lambda e, i=i, k=k, f=f, s=s, cs=cs: e.matmul(pGt[i][:], lhsT=Wg[s][:, k, f * 128:(f + 1) * 128], rhs=h2[:, k, cs], start=(k == 0), stop=(k == KT - 1)),
                                       reads=["Wg%d" % s] + h2keys, writes=["pGt%d" % i])
                            for k in range(KT):
                                S.emit("pe", lambda e, i=i, k=k, f=f, s=s, cs=cs: e.matmul(pUt[i][:], lhsT=Wu[s][:, k, f * 128:(f + 1) * 128], rhs=h2[:, k, cs], start=(k == 0), stop=(k == KT - 1)),
                                       reads=["Wu%d" % s] + h2keys, writes=["pUt%d" % i])
                            S.emit("act", lambda e, i=i: e.activation(out=sgt[i][:], in_=pGt[i][:], func=AF.Silu), reads=["pGt%d" % i], writes=["sgt%d" % i])
                            S.emit("dve", lambda e, i=i, f=f, s=s, cs=cs: e.tensor_tensor(out=hid[s][:, f, cs], in0=pUt[i][:], in1=sgt[i][:], op=ALU.mult),
                                   reads=["pUt%d" % i, "sgt%d" % i], writes=[("hid%d" % s, f, n)])

                        def emit_dn(ex, t, hf):
                            s = ex % 2
                            n = t // 4
                            i = cnt["dn"] % 4
                            cnt["dn"] += 1
                            for f in range(2):
                                S.emit("pe", lambda e, i=i, f=f, t=t, hf=hf, s=s: e.matmul(pDn[i][:], lhsT=hid[s][:, f, t * 128:(t + 1) * 128], rhs=Wd[s][:, f, hf * 512:(hf + 1) * 512],
                                                                                       start=(f == 0), stop=(f == 1)),
                                       reads=[("hid%d" % s, f, n), "Wd%d" % s], writes=["pDn%d" % i])
                            S.emit("dve", lambda e, i=i, t=t, hf=hf, ex=ex: e.scalar_tensor_tensor(out=acc[:, t, hf * 512:(hf + 1) * 512], in0=pDn[i][:], scalar=comb[:, t, ex:ex + 1],
                                                                                               in1=acc[:, t, hf * 512:(hf + 1) * 512], op0=ALU.mult, op1=ALU.add),
                                   reads=["pDn%d" % i, "comb", ("acc", t)], writes=[("acc", t)])

                        dma("sp", nw3bc[:], nw3_d.partition_broadcast(128), writes=["nw3bc"])
                        dma("sp", nwfbc[:], nwf_d.partition_broadcast(128), writes=["nwfbc"])
                        for ex in range(33):
                            if ex == 2:
                                dma("pool", Wpg[:], wpg_d.rearrange("(k p) c -> p k c", p=128), writes=["Wpg"])
                                dma("pool", Wple[:], wple_d.rearrange("(j p) c -> p j c", p=128), writes=["Wple"])
                            if ex < 32:
                                s = ex % 2
                                dma("pool", Wg[s][:], wg_d[ex].rearrange("(k p) f -> p k f", p=128), writes=["Wg%d" % s])
                                dma("pool", Wu[s][:], wu_d[ex].rearrange("(k p) f -> p k f", p=128), writes=["Wu%d" % s])
                                dma("pool", Wd[s][:], wd_d[ex].rearrange("(j p) c -> p j c", p=128), writes=["Wd%d" % s])
                            for n in range(PC_):
                                for f in range(2):
                                    if ex < 32:
                                        emit_gu(ex, n, f)
                                    if ex >= 1:
                                        for t in (4 * n + 2 * f, 4 * n + 2 * f + 1):
                                            for hf in range(2):
                                                emit_dn(ex - 1, t, hf)
                        S.barrier()
                        S.flush()
                    with ExitStack() as ph:
                        junk4 = sbt(ph, "junk4", [128, D], BF16)
                        ss3 = [sbt(ph, "ss3_%d" % i, [128, 1], F32) for i in range(2)]
                        rs3 = [sbt(ph, "rs3_%d" % i, [128, 1], F32) for i in range(2)]
                        ssf = [sbt(ph, "ssf_%d" % i, [128, 1], F32) for i in range(2)]
                        rsf = [sbt(ph, "rsf_%d" % i, [128, 1], F32) for i in range(2)]
                        h3 = [sbt(ph, "h3_%d" % i, [128, D], BF16) for i in range(2)]
                        h3T = [sbt(ph, "h3T%d" % i, [128, KT, 128], BF16) for i in range(2)]
                        pin = [sbt(ph, "pin%d" % i, [128, 256], F32) for i in range(2)]
                        pbf = [sbt(ph, "pbf%d" % i, [128, 256], BF16) for i in range(2)]
                        ppT = [sbt(ph, "ppT%d" % i, [128, 2, 128], BF16) for i in range(2)]
                        sgp = [sbt(ph, "sgp%d" % i, [128, D], F32) for i in range(2)]
                        x3 = [sbt(ph, "x3_%d" % i, [128, D], F32) for i in range(2)]
                        ot = [sbt(ph, "ot%d" % i, [128, D], F32) for i in range(2)]
                        pT3 = pst(ph, "pT3", [128, KT, 128], BF16)
                        pTp = pst(ph, "pTp", [128, KT, 128], BF16)
                        pGp = [pst(ph, "pGp%d" % i, [128, 512], F32) for i in range(2)]
                        pPp = [pst(ph, "pPp%d" % i, [128, 512], F32) for i in range(2)]
                        def p_s1(t):
                            s = t % 2
                            K_ = lambda nm, s=s: "%s%d" % (nm, s)
                            rows = slice(tok0 + t * 128, tok0 + (t + 1) * 128)
                            dma("sp", pin[s][:], p_d[rows, :], writes=[K_("pin")])
                            S.emit("act", lambda e, s=s, t=t: e.activation(out=junk4[:], in_=acc[:, t, :], func=AF.Square, accum_out=ss3[s][:]), reads=[("acc", t)], writes=["junk4", K_("ss3_")])
                            rstd_pow(ss3[s][:], rs3[s][:], D, [K_("ss3_")], [K_("rs3_")])
                            S.emit("dve", lambda e, s=s, t=t: e.scalar_tensor_tensor(out=h3[s][:], in0=acc[:, t, :], scalar=rs3[s][:, 0:1], in1=nw3bc[:], op0=ALU.mult, op1=ALU.mult),
                                   reads=[("acc", t), K_("rs3_"), "nw3bc"], writes=[K_("h3_")])
                            S.emit("pool", lambda e, s=s: e.tensor_copy(out=pbf[s][:], in_=pin[s][:]), reads=[K_("pin")], writes=[K_("pbf")])

                        def p_s2(t):
                            s = t % 2
                            K_ = lambda nm, s=s: "%s%d" % (nm, s)
                            for k in range(KT):
                                S.emit("pe", lambda e, k=k, s=s: e.transpose(pT3[:, k, :], h3[s][:, k * 128:(k + 1) * 128], ident_b[:]), reads=[K_("h3_"), "ident_b"], writes=["pT3"])
                            S.emit("act", lambda e, s=s: e.copy(out=h3T[s][:], in_=pT3[:]), reads=["pT3"], writes=[K_("h3T")])
                            for j in range(2):
                                S.emit("pe", lambda e, j=j, s=s: e.transpose(pTp[:, j, :], pbf[s][:, j * 128:(j + 1) * 128], ident_b[:]), reads=[K_("pbf"), "ident_b"], writes=["pTp"])
                            S.emit("act", lambda e, s=s: e.copy(out=ppT[s][:], in_=pTp[:, 0:2, :]), reads=["pTp"], writes=[K_("ppT")])
                            for hf in range(2):
                                hs = slice(hf * 512, (hf + 1) * 512)
                                for k in range(KT):
                                    S.emit("pe", lambda e, k=k, hf=hf, s=s, hs=hs: e.matmul(pGp[hf][:], lhsT=h3T[s][:, k, :], rhs=Wpg[:, k, hs], start=(k == 0), stop=(k == KT - 1)),
                                           reads=[K_("h3T"), "Wpg"], writes=["pGp%d" % hf])
                                for j in range(2):
                                    S.emit("pe", lambda e, j=j, hf=hf, s=s, hs=hs: e.matmul(pPp[hf][:], lhsT=ppT[s][:, j, :], rhs=Wple[:, j, hs], start=(j == 0), stop=(j == 1)),
                                           reads=[K_("ppT"), "Wple"], writes=["pPp%d" % hf])
                                S.emit("act", lambda e, hf=hf, s=s, hs=hs: e.activation(out=sgp[s][:, hs], in_=pGp[hf][:], func=AF.Sigmoid), reads=["pGp%d" % hf], writes=[K_("sgp")])
                                S.emit("dve", lambda e, hf=hf, s=s, hs=hs: e.tensor_tensor(out=x3[s][:, hs], in0=pPp[hf][:], in1=sgp[s][:, hs], op=ALU.mult),
                                       reads=["pPp%d" % hf, K_("sgp")], writes=[K_("x3_")])
                            S.emit("pool", lambda e, s=s, t=t: e.tensor_tensor(out=x3[s][:], in0=x3[s][:], in1=acc[:, t, :], op=ALU.add), reads=[K_("x3_"), ("acc", t)], writes=[K_("x3_")])

                        def p_s3(t):
                            s = t % 2
                            K_ = lambda nm, s=s: "%s%d" % (nm, s)
                            rows = slice(tok0 + t * 128, tok0 + (t + 1) * 128)
                            S.emit("act", lambda e, s=s: e.activation(out=junk4[:], in_=x3[s][:], func=AF.Square, accum_out=ssf[s][:]), reads=[K_("x3_")], writes=["junk4", K_("ssf_")])
                            rstd_pow(ssf[s][:], rsf[s][:], D, [K_("ssf_")], [K_("rsf_")])
                            S.emit("dve", lambda e, s=s: e.scalar_tensor_tensor(out=ot[s][:], in0=x3[s][:], scalar=rsf[s][:, 0:1], in1=nwfbc[:], op0=ALU.mult, op1=ALU.mult),
                                   reads=[K_("x3_"), K_("rsf_"), "nwfbc"], writes=[K_("ot")])
                            dma("sp", out_d[rows, :], ot[s][:], reads=[K_("ot")], writes=[("out", tok0 // 128 + t)])

                        for t in range(PT_ + 2):
                            if t < PT_:
                                p_s1(t)
                            if 1 <= t <= PT_:
                                p_s2(t - 1)
                            if t >= 2:
                                p_s3(t - 2)
                        S.barrier()
                        S.flush()
    return nc, dbg_out


def make_in_maps(inputs, L, cores):
    c = host_consts(L)
    f = lambda a: np.ascontiguousarray(a, dtype=np.float32)
    cw = inputs["conv_w"][0]
    cw_l = np.ascontiguousarray(cw.reshape(4, 10, 128).transpose(2, 1, 0).reshape(128, 40))
    cb_l = np.ascontiguousarray(inputs["conv_b"][0].reshape(10, 128).T)
    shared = {
        "w_in": f(inputs["w_in"][0]),
        "attn_norm_w": f(inputs["attn_norm_w"][0:1]),
        "conv_w": f(cw_l), "conv_b": f(cb_l),
        "dt_bias": f(inputs["dt_bias"][0:1]), "a_log": f(inputs["a_log"][0:1]), "d_skip": f(inputs["d_skip"][0:1]),
        "ssd_norm_w": f(inputs["ssd_norm_w"][0:1]),
        "w_ssd_out": f(inputs["w_ssd_out"][0]), "w_attn_out": f(inputs["w_attn_out"][0]), "w_out": f(inputs["w_out"][0]),
        "moe_norm_w": f(inputs["moe_norm_w"][0:1]),
        "w_rt": f(np.concatenate([inputs["w_router_group"][0], inputs["w_router_expert"][0]], axis=1)),
        "b_rt": f(np.concatenate([inputs["b_router_group"][0], inputs["b_router_expert"][0]])[None, :]),
        "w_exp_gate": f(inputs["w_exp_gate"][0].reshape(32, D, 256)),
        "w_exp_up": f(inputs["w_exp_up"][0].reshape(32, D, 256)),
        "w_exp_down": f(inputs["w_exp_down"][0].reshape(32, 256, D)),
        "ple_norm_w": f(inputs["ple_norm_w"][0:1]),
        "w_ple": f(inputs["w_ple"][0]), "w_ple_gate": f(inputs["w_ple_gate"][0]),
        "final_norm_w": f(inputs["final_norm_w"][None, :]),
    }
    for k, v in c.items():
        shared["c_" + k] = v
    maps = []
    for b in cores:
        m = dict(shared)
        m["x"] = f(inputs["x"][b, :L])
        m["p"] = f(inputs["p"][0, b, :L])
        m["pos"] = np.ascontiguousarray(inputs["positions"][b:b + 1, :L].astype(np.int32))
        maps.append(m)
    return maps


def kernel(**inputs):
    L = inputs["x"].shape[1]
    nc, _ = build(L)
    maps = make_in_maps(inputs, L, list(range(8)))
    res = run_bass_kernel_spmd(nc, maps, core_ids=list(range(8)))
    return np.stack([r["out"] for r in res.results], axis=0).astype(np.float32)
```

```python
import bisect
import math
from contextlib import ExitStack

import numpy as np
import concourse.bass as bass
import concourse.mybir as mybir
from concourse.bass_utils import run_bass_kernel_spmd

F32 = mybir.dt.float32
BF16 = mybir.dt.bfloat16
I32 = mybir.dt.int32
AF = mybir.ActivationFunctionType
ALU = mybir.AluOpType
AX = mybir.AxisListType

ENGS = ("pe", "act", "dve", "pool", "sp")
D = 1024
KT = 8
EPS = 1e-6
BIG = 32768.0
PI = math.pi


class Sched:
    def __init__(self, nc, n_dma_ch=40):
        self.nc = nc
        self.ops = {e: [] for e in ENGS}
        self.sig = {e: [] for e in ENGS}
        self.flushed = {e: 0 for e in ENGS}
        self.res = {}
        self.waited = {e: {} for e in ENGS}
        self.sems = {}
        self.dma_cnt = {}
        self.dma_last = {}
        self.n_dma_ch = n_dma_ch
        self.rr = 0
        self.last_real = {e: None for e in ENGS}
        self.gseq = 0
        self.dma_g = {}

    def alloc_sems(self, stack):
        for e in ENGS:
            self.sems[e] = stack.enter_context(self.nc.semaphore("s_" + e))
        for i in range(self.n_dma_ch):
            k = "d%d" % i
            self.sems[k] = stack.enter_context(self.nc.semaphore("s_" + k))
            self.dma_cnt[k] = 0
            self.dma_last[k] = None

    def _resolve(self, tok):
        if tok[0] == "d":
            return tok[1], tok[2]
        _, e, i = tok
        lst = self.sig[e]
        j = bisect.bisect_left(lst, i)
        if j == len(lst):
            last = self.last_real[e]
            assert last is not None and last >= i and last >= self.flushed[e], (e, i, last)
            self.ops[e][last]["sig"] = True
            lst.append(last)
        return e, j + 1

    def mark(self, i):
        pass

    def emit(self, eng, fn, reads=(), writes=(), dma_ch=None):
        if getattr(self, "drop", False):
            return None
        deps = []
        for r in reads:
            st = self.res.get(r)
            if st and st[0] is not None:
                deps.append((st[0], True))
        for w in writes:
            st = self.res.get(w)
            if st:
                if st[0] is not None:
                    deps.append((st[0], False))
                for rd in st[1]:
                    deps.append((rd, False))
        is_dma = dma_ch is not None
        if is_dma and self.dma_last[dma_ch] is not None:
            deps.append((self.dma_last[dma_ch], True))
        waits = {}
        wseq = {}
        for tok, raw in deps:
            if tok[0] == "c" and tok[1] == eng and not is_dma and not raw:
                continue
            k, v = self._resolve(tok)
            if self.waited[eng].get(k, 0) >= v:
                continue
            if waits.get(k, 0) < v:
                waits[k] = v
            g = self.ops[tok[1]][tok[2]]["g"] if tok[0] == "c" else self.dma_g.get((tok[1], tok[2]), 0)
            if wseq.get(k, -1) < g:
                wseq[k] = g
        for k, v in waits.items():
            self.waited[eng][k] = v
        idx = len(self.ops[eng])
        wl = sorted(waits.items(), key=lambda kv: wseq.get(kv[0], 0))
        self.gseq += 1
        rec = {"fn": fn, "waits": wl, "sig": False, "dma": None, "g": self.gseq}
        if is_dma:
            self.dma_cnt[dma_ch] += 16
            me = ("d", dma_ch, self.dma_cnt[dma_ch])
            rec["dma"] = dma_ch
            self.dma_last[dma_ch] = me
            self.dma_g[(dma_ch, self.dma_cnt[dma_ch])] = self.gseq
        else:
            me = ("c", eng, idx)
            self.last_real[eng] = idx
        self.ops[eng].append(rec)
        for r in reads:
            self.res.setdefault(r, [None, []])[1].append(me)
        for w in writes:
            self.res[w] = [me, []]
        return me

    def ch(self):
        k = "d%d" % (self.rr % self.n_dma_ch)
        self.rr += 1
        return k

    def barrier(self):
        toks = []
        for e in ENGS:
            if self.last_real[e] is not None:
                toks.append(("c", e, self.last_real[e]))
        for k, t in self.dma_last.items():
            if t is not None:
                toks.append(t)
        res = [self._resolve(t) for t in toks]
        for e in ENGS:
            waits = {}
            for k, v in res:
                if k == e:
                    continue
                if self.waited[e].get(k, 0) >= v:
                    continue
                waits[k] = max(waits.get(k, 0), v)
            for k, v in waits.items():
                self.waited[e][k] = v
            if waits:
                self.ops[e].append({"fn": None, "waits": list(waits.items()),
                                    "sig": False, "dma": None})
        self.res = {}

    def flush(self):
        nc = self.nc
        S = self

        def replay(name, eng):
            ops = S.ops[name]
            for rec in ops[S.flushed[name]:]:
                waits = rec["waits"]
                fuse = rec["fn"] is not None and len(waits) > 0
                for k, v in (waits[:-1] if fuse else waits):
                    eng.wait_ge(S.sems[k], v)
                if rec["fn"] is None:
                    continue
                ins = rec["fn"](eng)
                if fuse:
                    ins._wait_ge(S.sems[waits[-1][0]], waits[-1][1])
                if rec["dma"] is not None:
                    ins.then_inc(S.sems[rec["dma"]], 16)
                elif rec["sig"]:
                    ins.then_inc(S.sems[name], 1)
            S.flushed[name] = len(ops)

        with nc.Block() as block:
            @block.tensor
            def _(e):
                replay("pe", e)

            @block.scalar
            def _(e):
                replay("act", e)

            @block.vector
            def _(e):
                replay("dve", e)

            @block.gpsimd
            def _(e):
                replay("pool", e)

            @block.sync
            def _(e):
                replay("sp", e)


OFF_Z, OFF_XBC, OFF_DT, OFF_Q, OFF_K, OFF_V, OFF_GS, OFF_GA = 0, 1024, 2304, 2320, 3344, 4368, 5392, 6416


def host_consts(L=4096):
    c = {}
    c["ident"] = np.eye(128, dtype=np.float32)
    i = np.arange(128)
    c["T32"] = (i[:, None] <= i[None, :]).astype(np.float32)
    c["U32"] = (i[:, None] > i[None, :]).astype(np.float32)
    half = 32
    inv = (10000.0 ** (-np.arange(half, dtype=np.float32) / half)).astype(np.float32)
    c["invf"] = inv[i % 32].reshape(128, 1).astype(np.float32)
    c["sgn"] = np.where((i % 64) < 32, -1.0, 1.0).reshape(128, 1).astype(np.float32)
    pm = np.zeros((128, 128), np.float32)
    for p in range(128):
        src = p + 32 if (p % 64) < 32 else p - 32
        pm[src, p] = 1.0
    c["perm"] = pm
    oh = np.zeros((16, 16, 128), np.float32)
    for n in range(16):
        oh[n, n, :] = 1.0
    c["oh"] = oh.reshape(16, 16 * 128)
    kk = np.arange(4096)
    c["blkoh"] = (kk[None, :] // 256 == np.arange(16)[:, None]).astype(np.float32)
    NT = L // 128
    own = (np.arange(NT) // 2)[:, None, None]
    nn = np.arange(16)[None, None, :]
    z2 = np.zeros((NT, 2, 16), np.float32)
    c["gA"] = (z2 + np.where(nn >= own, -1e30, 0.0)).reshape(1, NT * 32).astype(np.float32)
    c["gM"] = (z2 + np.where(nn < own, 1.0, 0.0)).reshape(1, NT * 32).astype(np.float32)
    c["gO"] = (z2 + np.where(nn == own, 1.0, 0.0)).reshape(1, NT * 32).astype(np.float32)
    cm = np.zeros((128, 4, 512), np.float32)
    for r in range(4):
        for j in range(512):
            same = (r < 2) == (j < 256)
            kl = r * 128 + i
            cm[:, r, j] = np.where(same & (kl > j), -BIG, 0.0)
    c["cmask"] = cm.reshape(128, 2048)
    return c


def build(L, upto=99, debug=()):
    NT = L // 128
    NCH = L // 512
    NB = L // 256
    nc = bass.Bass("TRN2", target_bir_lowering=False)
    dbg_out = {}

    def din(name, shape, dt=F32):
        return nc.dram_tensor(name, shape, dt, kind="ExternalInput").ap()

    def dscr(name, shape, dt):
        kind = "ExternalOutput" if name in debug else "Internal"
        t = nc.dram_tensor(name, shape, dt, kind=kind).ap()
        if name in debug:
            dbg_out[name] = t
        return t

    x_d = din("x", [L, D])
    p_d = din("p", [L, 256])
    pos_d = din("pos", [1, L], I32)
    w_in_d = din("w_in", [D, 7440])
    nw1_d = din("attn_norm_w", [1, D])
    cw_d = din("conv_w", [128, 40])
    cb_d = din("conv_b", [128, 10])
    dtb_d = din("dt_bias", [1, 16])
    alog_d = din("a_log", [1, 16])
    dsk_d = din("d_skip", [1, 16])
    snw_d = din("ssd_norm_w", [1, D])
    wso_d = din("w_ssd_out", [D, D])
    wao_d = din("w_attn_out", [D, D])
    wo_d = din("w_out", [D, D])
    nw2_d = din("moe_norm_w", [1, D])
    wrt_d = din("w_rt", [D, 36])
    brt_d = din("b_rt", [1, 36])
    wg_d = din("w_exp_gate", [32, D, 256])
    wu_d = din("w_exp_up", [32, D, 256])
    wd_d = din("w_exp_down", [32, 256, D])
    nw3_d = din("ple_norm_w", [1, D])
    wple_d = din("w_ple", [256, D])
    wpg_d = din("w_ple_gate", [D, D])
    nwf_d = din("final_norm_w", [1, D])
    c_ident_d = din("c_ident", [128, 128])
    c_T_d = din("c_T32", [128, 128])
    c_U_d = din("c_U32", [128, 128])
    c_invf_d = din("c_invf", [128, 1])
    c_sgn_d = din("c_sgn", [128, 1])
    c_perm_d = din("c_perm", [128, 128])
    c_oh_d = din("c_oh", [16, 2048])
    c_blkoh_d = din("c_blkoh", [16, 4096])
    c_gA_d = din("c_gA", [1, NT * 32])
    c_gM_d = din("c_gM", [1, NT * 32])
    c_gO_d = din("c_gO", [1, NT * 32])
    c_cm_d = din("c_cmask", [128, 2048])
    out_d = nc.dram_tensor("out", [L, D], F32, kind="ExternalOutput").ap()

    xbcT_s = dscr("xbcT_s", [1280, L], BF16)
    qT_s = dscr("qT_s", [D, L], BF16)
    kT_s = dscr("kT_s", [D, L], BF16)
    sgT_s = dscr("sgT_s", [2 * D, L], BF16)
    zs_s = dscr("zs_s", [L, D], BF16)
    vaug_s = dscr("vaug_s", [L, 16 * 65], BF16)
    dt_s = dscr("dt_s", [128, NT * 16], F32)
    ssdT_s = dscr("ssdT_s", [D, L], BF16)
    attT_s = dscr("attT_s", [D, L], BF16)
    x1_s = dscr("x1_s", [L, D], F32)

    top = ExitStack()
    with top:
        S = Sched(nc)
        S.alloc_sems(top)
        lp = top.enter_context(nc.allow_low_precision("bf16 matmul operands, fp32 accumulation"))

        uniq = [0]

        def sbt(st, name, shape, dt):
            uniq[0] += 1
            return st.enter_context(nc.sbuf_tensor("%s_u%d" % (name, uniq[0]), shape, dt))

        def pst(st, name, shape, dt):
            uniq[0] += 1
            return st.enter_context(nc.psum_tensor("%s_u%d" % (name, uniq[0]), shape, dt))

        def dma(q, out, in_, reads=(), writes=()):
            return S.emit(q, lambda e: e.dma_start(out=out, in_=in_), reads=reads, writes=writes, dma_ch=S.ch())

        ident_f = sbt(top, "ident_f", [128, 128], F32)
        ident_b = sbt(top, "ident_b", [128, 128], BF16)
        dtall = sbt(top, "dtall", [128, NT, 16], F32)
        dma("sp", ident_f[:], c_ident_d, writes=["ident_f"])
        dma("pool", ident_b[:], c_ident_d, writes=["ident_b"])

        def rstd_from_ss(ss_ap, rstd_ap, n, keys_r, keys_w):
            S.emit("act", lambda e: e.activation(out=rstd_ap, in_=ss_ap, func=AF.Sqrt, scale=1.0 / n, bias=EPS_T[:, 0:1]),
                   reads=list(keys_r) + ["eps_t"], writes=keys_w)
            S.emit("dve", lambda e: e.reciprocal(out=rstd_ap, in_=rstd_ap), reads=keys_w, writes=keys_w)

        NHALF = sbt(top, "nhalf", [128, 1], F32)
        S.emit("pool", lambda e: e.memset(NHALF[:], -0.5), writes=["nhalf"])

        def rstd_pow(ss_ap, rstd_ap, n, keys_r, keys_w):
            S.emit("dve", lambda e: e.tensor_scalar(out=rstd_ap, in0=ss_ap, scalar1=1.0 / n, scalar2=EPS, op0=ALU.mult, op1=ALU.add),
                   reads=list(keys_r), writes=keys_w)
            S.emit("pool", lambda e: e.tensor_tensor(out=rstd_ap, in0=rstd_ap, in1=NHALF[:, 0:1], op=ALU.pow), reads=keys_w + ["nhalf"], writes=keys_w)

        EPS_T = sbt(top, "eps_t", [128, 1], F32)
        S.emit("pool", lambda e: e.memset(EPS_T[:], EPS), writes=["eps_t"])

        with ExitStack() as ph:
            hT = sbt(ph, "hT", [128, KT, L], BF16)
            cosT = sbt(ph, "cosT", [128, L], F32)
            sinT = sbt(ph, "sinT", [128, L], F32)
            rtmp = sbt(ph, "rtmp", [128, L], F32)
            rtmp2 = sbt(ph, "rtmp2", [128, L], F32)
            rti = sbt(ph, "rti", [128, L], I32)
            nwbc = sbt(ph, "nwbc", [128, D], F32)
            invf = sbt(ph, "invf", [128, 1], F32)
            sgn = sbt(ph, "sgn", [128, 1], F32)
            perm = sbt(ph, "perm", [128, 128], BF16)
            xin = [sbt(ph, "xin%d" % i, [128, D], F32) for i in range(3)]
            hn = [sbt(ph, "hn%d" % i, [128, D], BF16) for i in range(3)]
            junk = sbt(ph, "junk", [128, D], BF16)
            ss = [sbt(ph, "ss%d" % i, [128, 1], F32) for i in range(3)]
            rs = [sbt(ph, "rs%d" % i, [128, 1], F32) for i in range(3)]
            wbf = [sbt(ph, "wbf%d" % i, [128, KT, 512], BF16) for i in range(2)]
            stg = [sbt(ph, "stg%d" % i, [128, 512], BF16) for i in range(4)]
            vst = [sbt(ph, "vst%d" % i, [128, 8, 65], BF16) for i in range(2)]
            qsb = [sbt(ph, "qsb%d" % i, [128, 512], BF16) for i in range(2)]
            t1 = [sbt(ph, "t1_%d" % i, [128, 512], F32) for i in range(2)]
            t2 = [sbt(ph, "t2_%d" % i, [128, 512], F32) for i in range(2)]
            pp = [pst(ph, "pp%d" % i, [128, 512], F32) for i in range(4)]
            pT = [pst(ph, "pT%d" % i, [128, KT, 128], BF16) for i in range(2)]
            prot = [pst(ph, "prot%d" % i, [128, 512], F32) for i in range(2)]

            dma("sp", nwbc[:], nw1_d.partition_broadcast(128), writes=["nwbc"])
            dma("sp", invf[:], c_invf_d, writes=["invf"])
            dma("sp", sgn[:], c_sgn_d, writes=["sgn"])
            dma("pool", perm[:], c_perm_d, writes=["perm"])
            dma("sp", rti[:], pos_d.partition_broadcast(128), writes=["rti"])
            for i in range(2):
                S.emit("pool", lambda e, i=i: e.memset(vst[i][:, :, 64:65], 1.0), writes=["vst%d" % i])

            def n1_s1(t):
                s = t % 3
                dma("sp", xin[s][:], x_d[t * 128:(t + 1) * 128, :], writes=["xin%d" % s])
                S.emit("act", lambda e, s=s: e.activation(out=junk[:], in_=xin[s][:], func=AF.Square, accum_out=ss[s][:]),
                       reads=["xin%d" % s], writes=["junk", "ss%d" % s])
                rstd_from_ss(ss[s][:], rs[s][:], D, ["ss%d" % s], ["rs%d" % s])
                S.emit("dve", lambda e, s=s: e.scalar_tensor_tensor(out=hn[s][:], in0=xin[s][:], scalar=rs[s][:, 0:1], in1=nwbc[:],
                                                                   op0=ALU.mult, op1=ALU.mult),
                       reads=["xin%d" % s, "rs%d" % s, "nwbc"], writes=["hn%d" % s])

            def n1_s2(t):
                s = t % 3
                u = t % 2
                for k in range(KT):
                    S.emit("pe", lambda e, s=s, k=k, u=u: e.transpose(pT[u][:, k, :], hn[s][:, k * 128:(k + 1) * 128], ident_b[:]),
                           reads=["hn%d" % s, "ident_b"], writes=["pT%d" % u])
                S.emit("act", lambda e, u=u, t=t: e.copy(out=hT[:, :, t * 128:(t + 1) * 128], in_=pT[u][:]),
                       reads=["pT%d" % u], writes=[("hT", t)])

            for t in range(NT + 1):
                if t < NT:
                    n1_s1(t)
                if t >= 1:
                    n1_s2(t - 1)

            S.emit("dve", lambda e: e.tensor_copy(out=rtmp[:], in_=rti[:]), reads=["rti"], writes=["rtmp"])
            S.emit("dve", lambda e: e.tensor_scalar(out=rtmp[:], in0=rtmp[:], scalar1=invf[:, 0:1], scalar2=None, op0=ALU.mult),
                   reads=["rtmp", "invf"], writes=["rtmp"])

            def sin_table(dst, shift, key):
                S.emit("dve", lambda e: e.tensor_scalar(out=rtmp2[:], in0=rtmp[:], scalar1=shift, scalar2=1.0 / (2 * PI),
                                                        op0=ALU.add, op1=ALU.mult), reads=["rtmp"], writes=["rtmp2"])
                S.emit("dve", lambda e: e.tensor_copy(out=rti[:], in_=rtmp2[:]), reads=["rtmp2"], writes=["rti"])
                S.emit("dve", lambda e: e.tensor_copy(out=rtmp2[:], in_=rti[:]), reads=["rti"], writes=["rtmp2"])
                S.emit("dve", lambda e: e.tensor_scalar(out=rtmp2[:], in0=rtmp2[:], scalar1=-2 * PI, scalar2=shift,
                                                        op0=ALU.mult, op1=ALU.add), reads=["rtmp2"], writes=["rtmp2"])
                S.emit("dve", lambda e: e.tensor_tensor(out=rtmp2[:], in0=rtmp2[:], in1=rtmp[:], op=ALU.add),
                       reads=["rtmp2", "rtmp"], writes=["rtmp2"])
                S.emit("dve", lambda e: e.tensor_scalar(out=dst[:], in0=rtmp2[:], scalar1=PI, scalar2=-2 * PI,
                                                        op0=ALU.is_gt, op1=ALU.mult), reads=["rtmp2"], writes=[key])
                S.emit("dve", lambda e: e.tensor_tensor(out=rtmp2[:], in0=rtmp2[:], in1=dst[:], op=ALU.add),
                       reads=["rtmp2", key], writes=["rtmp2"])
                S.emit("dve", lambda e: e.tensor_scalar(out=dst[:], in0=rtmp2[:], scalar1=-PI, scalar2=2 * PI,
                                                        op0=ALU.is_lt, op1=ALU.mult), reads=["rtmp2"], writes=[key])
                S.emit("dve", lambda e: e.tensor_tensor(out=rtmp2[:], in0=rtmp2[:], in1=dst[:], op=ALU.add),
                       reads=["rtmp2", key], writes=["rtmp2"])
                S.emit("dve", lambda e: e.tensor_scalar(out=rtmp2[:], in0=rtmp2[:], scalar1=PI, scalar2=-PI,
                                                        op0=ALU.min, op1=ALU.max), reads=["rtmp2"], writes=["rtmp2"])
                S.emit("act", lambda e: e.activation(out=dst[:], in_=rtmp2[:], func=AF.Sin), reads=["rtmp2"], writes=[key])

            sin_table(cosT, PI / 2, "cosT")
            sin_table(sinT, 0.0, "sinT")
            S.emit("dve", lambda e: e.tensor_scalar(out=sinT[:], in0=sinT[:], scalar1=sgn[:, 0:1], scalar2=None, op0=ALU.mult),
                   reads=["sinT", "sgn"], writes=["sinT"])

            st_i = [0]
            pp_i = [0]
            w_i = [0]

            def load_w(c0, w):
                s = w_i[0] % 2
                w_i[0] += 1
                dma("pool", wbf[s][:, :, 0:w], w_in_d[:, c0:c0 + w].rearrange("(k p) c -> p k c", p=128), writes=["wbf%d" % s])
                return s

            def mm_fm(ws, j, n):
                ps = pp_i[0] % 4
                pp_i[0] += 1
                for k in range(KT):
                    S.emit("pe", lambda e, k=k, ps=ps: e.matmul(pp[ps][:], lhsT=wbf[ws][:, k, j * 128:(j + 1) * 128],
                                                               rhs=hT[:, k, n * 512:(n + 1) * 512], start=(k == 0), stop=(k == KT - 1)),
                           reads=["wbf%d" % ws] + [("hT", 4 * n + i) for i in range(4)], writes=["pp%d" % ps])
                return ps

            def next_stg():
                s = st_i[0] % 4
                st_i[0] += 1
                return s

            fm_segs = [("xbc", OFF_XBC, 1280), ("q", OFF_Q, 1024), ("k", OFF_K, 1024), ("gs", OFF_GS, 1024), ("ga", OFF_GA, 1024)]
            ev_i = [0]
            for name, off, width in fm_segs:
                for b0 in range(0, width, 512):
                    bw = min(512, width - b0)
                    ws = load_w(off + b0, bw)
                    for j in range(bw // 128):
                        row0 = b0 + j * 128
                        for n in range(NCH):
                            ps = mm_fm(ws, j, n)
                            cols = slice(n * 512, (n + 1) * 512)
                            if name == "xbc":
                                sg = next_stg()
                                eng = "dve" if ev_i[0] % 2 == 0 else "act"
                                ev_i[0] += 1
                                if eng == "dve":
                                    S.emit("dve", lambda e, sg=sg, ps=ps: e.tensor_copy(out=stg[sg][:], in_=pp[ps][:]),
                                           reads=["pp%d" % ps], writes=["stg%d" % sg])
                                else:
                                    S.emit("act", lambda e, sg=sg, ps=ps: e.copy(out=stg[sg][:], in_=pp[ps][:]),
                                           reads=["pp%d" % ps], writes=["stg%d" % sg])
                                dma("sp", xbcT_s[row0:row0 + 128, cols], stg[sg][:], reads=["stg%d" % sg], writes=[("xbcT", row0 // 128)])
                            elif name in ("gs", "ga"):
                                sg = next_stg()
                                S.emit("act", lambda e, sg=sg, ps=ps: e.activation(out=stg[sg][:], in_=pp[ps][:], func=AF.Sigmoid),
                                       reads=["pp%d" % ps], writes=["stg%d" % sg])
                                r0 = (0 if name == "gs" else D) + row0
                                dma("sp", sgT_s[r0:r0 + 128, cols], stg[sg][:], reads=["stg%d" % sg], writes=[("sgT", r0 // 128)])
                            else:
                                qs = ev_i[0] % 2
                                ev_i[0] += 1
                                S.emit("act", lambda e, qs=qs, ps=ps: e.copy(out=qsb[qs][:], in_=pp[ps][:]),
                                       reads=["pp%d" % ps], writes=["qsb%d" % qs])
                                S.emit("pe", lambda e, qs=qs: e.matmul(prot[qs][:], lhsT=perm[:], rhs=qsb[qs][:], start=True, stop=True),
                                       reads=["perm", "qsb%d" % qs], writes=["prot%d" % qs])
                                S.emit("pool", lambda e, qs=qs, cols=cols: e.tensor_tensor(out=t1[qs][:], in0=qsb[qs][:], in1=cosT[:, cols], op=ALU.mult),
                                       reads=["qsb%d" % qs, "cosT"], writes=["t1_%d" % qs])
                                S.emit("dve", lambda e, qs=qs, cols=cols: e.tensor_tensor(out=t2[qs][:], in0=prot[qs][:], in1=sinT[:, cols], op=ALU.mult),
                                       reads=["prot%d" % qs, "sinT"], writes=["t2_%d" % qs])
                                sg = next_stg()
                                S.emit("dve", lambda e, qs=qs, sg=sg: e.tensor_tensor(out=stg[sg][:], in0=t1[qs][:], in1=t2[qs][:], op=ALU.add),
                                       reads=["t1_%d" % qs, "t2_%d" % qs], writes=["stg%d" % sg])
                                dst = qT_s if name == "q" else kT_s
                                dma("sp", dst[row0:row0 + 128, cols], stg[sg][:], reads=["stg%d" % sg], writes=[(name + "T", row0 // 128)])

            for name, off, width in (("z", OFF_Z, 1024), ("v", OFF_V, 1024), ("dt", OFF_DT, 16)):
                for b0 in range(0, width, 512):
                    bw = min(512, width - b0)
                    ws = load_w(off + b0, bw)
                    for t in range(NT):
                        ps = pp_i[0] % 4
                        pp_i[0] += 1
                        for k in range(KT):
                            S.emit("pe", lambda e, k=k, ps=ps, t=t, bw=bw, ws=ws: e.matmul(
                                pp[ps][:, 0:bw], lhsT=hT[:, k, t * 128:(t + 1) * 128], rhs=wbf[ws][:, k, 0:bw],
                                start=(k == 0), stop=(k == KT - 1)),
                                reads=["wbf%d" % ws, ("hT", t)], writes=["pp%d" % ps])
                        rows = slice(t * 128, (t + 1) * 128)
                        if name == "z":
                            sg = next_stg()
                            S.emit("act", lambda e, sg=sg, ps=ps: e.activation(out=stg[sg][:], in_=pp[ps][:], func=AF.Silu),
                                   reads=["pp%d" % ps], writes=["stg%d" % sg])
                            dma("sp", zs_s[rows, b0:b0 + 512], stg[sg][:], reads=["stg%d" % sg], writes=[("zs", t)])
                        elif name == "v":
                            vs = ev_i[0] % 2
                            ev_i[0] += 1
                            S.emit("dve", lambda e, vs=vs, ps=ps: e.tensor_copy(out=vst[vs][:, :, 0:64], in_=pp[ps][:].rearrange("p (h d) -> p h d", h=8)),
                                   reads=["pp%d" % ps], writes=["vst%d" % vs])
                            h0 = b0 // 64
                            dma("sp", vaug_s[rows, h0 * 65:(h0 + 8) * 65], vst[vs][:].rearrange("p h d -> p (h d)"),
                                reads=["vst%d" % vs], writes=[("vaug", t)])
                        else:
                            S.emit("dve", lambda e, ps=ps, t=t: e.tensor_copy(out=dtall[:, t, :], in_=pp[ps][:, 0:16]),
                                   reads=["pp%d" % ps], writes=["dtall"])
            if "dt_s" in debug:
                dma("sp", dt_s, dtall[:].rearrange("p t h -> p (t h)"), reads=["dtall"], writes=["dt_s"])
            S.barrier()
            S.flush()


        if upto >= 2:
            with ExitStack() as ph:
                xc = sbt(ph, "xc", [128, 10, L], BF16)
                T32 = sbt(ph, "T32", [128, 128], F32)
                U32 = sbt(ph, "U32", [128, 128], F32)
                ones32 = sbt(ph, "ones32", [128, 128], F32)
                ONE_T = sbt(ph, "one_t", [128, 1], F32)
                cw = sbt(ph, "cw", [128, 40], F32)
                cb = sbt(ph, "cb", [128, 10], F32)
                dtb_bc = sbt(ph, "dtb_bc", [128, 16], F32)
                aneg = sbt(ph, "aneg", [128, 16], F32)
                dsk_bc = sbt(ph, "dsk_bc", [128, 16], F32)
                snwbc = sbt(ph, "snwbc", [128, D], F32)
                dx = sbt(ph, "dx", [128, NT, 16], F32)
                dl = sbt(ph, "dl", [128, NT, 16], F32)
                dtv = sbt(ph, "dtv", [128, NT, 16], F32)
                dA = sbt(ph, "dA", [128, NT, 16], F32)
                dma("sp", T32[:], c_T_d, writes=["T32"])
                dma("sp", U32[:], c_U_d, writes=["U32"])
                dma("sp", cw[:], cw_d, writes=["cw"])
                dma("sp", cb[:], cb_d, writes=["cb"])
                dma("sp", dtb_bc[:], dtb_d.partition_broadcast(128), writes=["dtb_bc"])
                dma("sp", aneg[:], alog_d.partition_broadcast(128), writes=["aneg"])
                dma("sp", dsk_bc[:], dsk_d.partition_broadcast(128), writes=["dsk_bc"])
                dma("sp", snwbc[:], snw_d.partition_broadcast(128), writes=["snwbc"])
                S.emit("pool", lambda e: e.memset(ones32[:], 1.0), writes=["ones32"])
                S.emit("pool", lambda e: e.memset(ONE_T[:], 1.0), writes=["one_t"])
                S.emit("dve", lambda e: e.tensor_tensor(out=dx[:], in0=dtall[:], in1=dtb_bc[:].unsqueeze(1).to_broadcast([128, NT, 16]), op=ALU.add),
                       reads=["dtall", "dtb_bc"], writes=["dx"])
                S.emit("dve", lambda e: e.scalar_tensor_tensor(out=dl[:], in0=dx[:], scalar=-1.0, in1=dx[:], op0=ALU.mult, op1=ALU.max), reads=["dx"], writes=["dl"])
                S.emit("act", lambda e: e.activation(out=dl[:], in_=dl[:], func=AF.Exp, scale=-1.0), reads=["dl"], writes=["dl"])
                S.emit("act", lambda e: e.activation(out=dl[:], in_=dl[:], func=AF.Ln, bias=ONE_T[:, 0:1]), reads=["dl", "one_t"], writes=["dl"])
                S.emit("dve", lambda e: e.scalar_tensor_tensor(out=dtv[:], in0=dx[:], scalar=0.0, in1=dl[:], op0=ALU.max, op1=ALU.add),
                       reads=["dx", "dl"], writes=["dtv"])
                S.emit("act", lambda e: e.activation(out=aneg[:], in_=aneg[:], func=AF.Exp), reads=["aneg"], writes=["aneg"])
                S.emit("dve", lambda e: e.tensor_scalar(out=aneg[:], in0=aneg[:], scalar1=-1.0, scalar2=None, op0=ALU.mult), reads=["aneg"], writes=["aneg"])
                S.emit("dve", lambda e: e.tensor_tensor(out=dA[:], in0=dtv[:], in1=aneg[:].unsqueeze(1).to_broadcast([128, NT, 16]), op=ALU.mult),
                       reads=["dtv", "aneg"], writes=["dA"])

                with ExitStack() as ph1:
                    cin = [sbt(ph1, "cin%d" % i, [128, L + 3], BF16) for i in range(2)]
                    dg = [sbt(ph1, "dg%d" % i, [128, 4, 128], BF16) for i in range(2)]
                    pc = [pst(ph1, "pc%d" % i, [128, 512], F32) for i in range(4)]
                    for i in range(2):
                        S.emit("pool", lambda e, i=i: e.memset(cin[i][:, 0:3], 0.0), writes=["cin%d" % i])
                    pi = 0
                    for ct in range(10):
                        s = ct % 2
                        dma("sp", cin[s][:, 3:3 + L], xbcT_s[ct * 128:(ct + 1) * 128, :], reads=[("xbcT", ct)], writes=["cin%d" % s])
                        for kk in range(4):
                            S.emit("pool", lambda e, s=s, kk=kk, ct=ct: e.tensor_scalar(out=dg[s][:, kk, :], in0=ident_f[:], scalar1=cw[:, ct * 4 + kk:ct * 4 + kk + 1],
                                                                                     scalar2=None, op0=ALU.mult),
                                   reads=["ident_f", "cw"], writes=["dg%d" % s])
                        for n in range(NCH):
                            ps = pi % 4
                            pi += 1
                            for kk in range(4):
                                S.emit("pe", lambda e, s=s, kk=kk, n=n, ps=ps: e.matmul(pc[ps][:], lhsT=dg[s][:, kk, :], rhs=cin[s][:, n * 512 + kk:n * 512 + kk + 512],
                                                                                     start=(kk == 0), stop=(kk == 3)),
                                       reads=["dg%d" % s, "cin%d" % s], writes=["pc%d" % ps])
                            S.emit("act", lambda e, ct=ct, n=n, ps=ps: e.activation(out=xc[:, ct, n * 512:(n + 1) * 512], in_=pc[ps][:], func=AF.Silu, bias=cb[:, ct:ct + 1]),
                                   reads=["pc%d" % ps, "cb"], writes=[("xc", ct)])
                    S.barrier()
                    S.flush()

                if "xc_s" in debug:
                    xc_s = dscr("xc_s", [1280, L], BF16)
                    dma("sp", xc_s.rearrange("(c p) t -> p c t", p=128), xc[:], reads=[("xc", i) for i in range(10)], writes=["xc_s"])
                    S.barrier()
                    S.flush()
                with ExitStack() as ph2:
                    if upto < 3:
                        raise_skip = True
                    else:
                        raise_skip = False
                    Xdt = [sbt(ph2, "Xdt%d" % i, [128, D], BF16) for i in range(2)]
                    XD = [sbt(ph2, "XD%d" % i, [128, D], F32) for i in range(2)]
                    Xdec = [sbt(ph2, "Xdec%d" % i, [128, D], BF16) for i in range(2)]
                    Btok = [sbt(ph2, "Btok%d" % i, [128, 128], BF16) for i in range(2)]
                    CBm = [sbt(ph2, "CBm%d" % i, [128, 2, 128], F32) for i in range(2)]
                    exps = [sbt(ph2, "exps%d" % i, [128, 48], F32) for i in range(2)]
                    rhsD = [sbt(ph2, "rhsD%d" % i, [128, 16, 128], F32) for i in range(2)]
                    Lm = [sbt(ph2, "Lm%d" % i, [128, 512], F32) for i in range(2)]
                    MT = [sbt(ph2, "MT%d" % i, [128, 16, 128], BF16) for i in range(2)]
                    Hs = sbt(ph2, "Hs", [128, 512], F32)
                    Hbf = sbt(ph2, "Hbf", [128, 512], BF16)
                    yoffs = sbt(ph2, "yoffs", [128, D], F32)
                    ysb = [sbt(ph2, "ysb%d" % i, [128, D], F32) for i in range(2)]
                    zt = [sbt(ph2, "zt%d" % i, [128, D], BF16) for i in range(2)]
                    yn = [sbt(ph2, "yn%d" % i, [128, D], BF16) for i in range(2)]
                    junk2 = sbt(ph2, "junk2", [128, 512], BF16)
                    ss2 = [sbt(ph2, "ss2_%d" % i, [128, 2], F32) for i in range(2)]
                    rs2 = [sbt(ph2, "rs2_%d" % i, [128, 2], F32) for i in range(2)]
                    stgT = [sbt(ph2, "stgT%d" % i, [128, KT, 512], BF16) for i in range(2)]
                    psm = pst(ph2, "psm", [128, 512], F32)
                    pX = pst(ph2, "pX", [128, KT, 128], BF16)
                    pBt_full = pst(ph2, "pBt", [128, 1024], BF16)
                    pBt = pBt_full[:, 0:128]
                    pD = [pst(ph2, "pD%d" % i, [128, 512], F32) for i in range(2)]
                    pY = [pst(ph2, "pY%d" % i, [128, 512], F32) for i in range(2)]
                    pO = pst(ph2, "pO", [128, 512], F32)
                    Cz = sbt(ph2, "Cz", [128, 2, L], BF16)
                    S.emit("pool", lambda e: e.memset(Cz[:], 0.0), writes=["Cz"])
                    S.emit("act", lambda e: e.copy(out=Cz[0:64, 0, :], in_=xc[0:64, 9, :]), reads=[("xc", 9), "Cz"], writes=["Cz"])
                    S.emit("dve", lambda e: e.tensor_copy(out=Cz[64:128, 1, :], in_=xc[64:128, 9, :]), reads=[("xc", 9), "Cz"], writes=["Cz"])
                    S.emit("pool", lambda e: e.memset(Hs[:], 0.0), writes=["Hs"])
                    S.emit("pool", lambda e: e.memset(Hbf[:], 0.0), writes=["Hbf"])
                    def stage1a(c):
                        s = c % 2
                        cols = slice(c * 128, (c + 1) * 128)
                        K_ = lambda n, s=s: "%s%d" % (n, s)
                        S.emit("pe", lambda e, c=c: e.matmul(psm[:, 0:16], lhsT=T32[:], rhs=dA[:, c, :], start=True, stop=True), reads=["T32", "dA"], writes=["psmA"])
                        S.emit("pe", lambda e, c=c: e.matmul(psm[:, 16:32], lhsT=U32[:], rhs=dA[:, c, :], start=True, stop=True), reads=["U32", "dA"], writes=["psmA"])
                        S.emit("pe", lambda e, c=c: e.matmul(psm[:, 32:48], lhsT=ones32[:], rhs=dA[:, c, :], start=True, stop=True), reads=["ones32", "dA"], writes=["psmA"])
                        S.emit("act", lambda e, s=s: e.activation(out=exps[s][:], in_=psm[:, 0:48], func=AF.Exp), reads=["psmA"], writes=[K_("exps")])
                        S.emit("pool", lambda e, s=s, c=c: e.tensor_tensor(out=rhsD[s][:], in0=T32[:].unsqueeze(1).to_broadcast([128, 16, 128]),
                                                                          in1=dA[:, c, :].unsqueeze(2).to_broadcast([128, 16, 128]), op=ALU.mult),
                               reads=["T32", "dA"], writes=[K_("rhsD")])
                        S.mark(1)
                        for k in range(KT):
                            S.emit("pe", lambda e, k=k, cols=cols: e.transpose(pX[:, k, :], xc[:, k, cols], ident_b[:]), reads=[("xc", k), "ident_b"], writes=["pX"])
                        S.emit("pe", lambda e, cols=cols: e.transpose(pBt, xc[:, 8, cols], ident_b[:]), reads=[("xc", 8), "ident_b"], writes=["pBt"])
                        S.emit("dve", lambda e, s=s, c=c: e.tensor_tensor(out=Xdt[s][:].rearrange("p (h d) -> p h d", h=16), in0=pX[:].rearrange("p k (a d) -> p (k a) d", a=2),
                                                                         in1=dtv[:, c, :].unsqueeze(2).to_broadcast([128, 16, 64]), op=ALU.mult),
                               reads=["pX", "dtv"], writes=[K_("Xdt")])
                        S.emit("dve", lambda e, s=s: e.tensor_tensor(out=XD[s][:].rearrange("p (h d) -> p h d", h=16), in0=pX[:].rearrange("p k (a d) -> p (k a) d", a=2),
                                                                    in1=dsk_bc[:].unsqueeze(2).to_broadcast([128, 16, 64]), op=ALU.mult),
                               reads=["pX", "dsk_bc"], writes=[K_("XD")])
                        S.emit("act", lambda e, s=s: e.copy(out=Btok[s][:], in_=pBt), reads=["pBt"], writes=[K_("Btok")])
                        S.mark(2)
                        for g in range(2):
                            S.emit("pe", lambda e, g=g, cols=cols: e.matmul(psm[:, 64 + g * 128:64 + (g + 1) * 128], lhsT=xc[:, 8, cols],
                                                                          rhs=Cz[:, g, cols], start=True, stop=True),
                                   reads=[("xc", 8), "Cz"], writes=["psmCB"])
                        S.emit("dve", lambda e, s=s: e.tensor_tensor(out=CBm[s][:], in0=psm[:, 64:320].rearrange("p (g l) -> p g l", g=2),
                                                                    in1=T32[:].unsqueeze(1).to_broadcast([128, 2, 128]), op=ALU.mult),
                               reads=["psmCB", "T32"], writes=[K_("CBm")])
                        S.mark(3)
                        for j in range(4):
                            jj = j % 2
                            S.emit("pe", lambda e, s=s, j=j, jj=jj: e.matmul(pD[jj][:], lhsT=U32[:], rhs=rhsD[s][:, 4 * j:4 * j + 4, :].rearrange("p h l -> p (h l)"),
                                                                            start=True, stop=True), reads=["U32", K_("rhsD")], writes=["pD%d" % jj])
                            S.emit("act", lambda e, jj=jj: e.activation(out=Lm[jj][:], in_=pD[jj][:], func=AF.Exp), reads=["pD%d" % jj], writes=["Lm%d" % jj])
                            S.emit("dve", lambda e, s=s, j=j, jj=jj: e.tensor_tensor(out=MT[s][:, 4 * j:4 * j + 4, :], in0=Lm[jj][:].rearrange("p (h l) -> p h l", h=4),
                                                                                    in1=CBm[s][:, j // 2, :].unsqueeze(1).to_broadcast([128, 4, 128]), op=ALU.mult),
                                   reads=["Lm%d" % jj, K_("CBm")], writes=[K_("MT")])
                        S.mark(4)
                        S.emit("dve", lambda e, s=s: e.tensor_tensor(out=Xdec[s][:].rearrange("p (h d) -> p h d", h=16), in0=Xdt[s][:].rearrange("p (h d) -> p h d", h=16),
                                                                    in1=exps[s][:, 16:32].unsqueeze(2).to_broadcast([128, 16, 64]), op=ALU.mult),
                               reads=[K_("Xdt"), K_("exps")], writes=[K_("Xdec")])

                    def stage1b(c):
                        s = c % 2
                        cols = slice(c * 128, (c + 1) * 128)
                        K_ = lambda n, s=s: "%s%d" % (n, s)
                        for h in range(16):
                            S.emit("pe", lambda e, s=s, h=h: e.matmul(pY[h // 8][:, (h % 8) * 64:(h % 8 + 1) * 64], lhsT=MT[s][:, h, :], rhs=Xdt[s][:, h * 64:(h + 1) * 64],
                                                                     start=True, stop=True), reads=[K_("MT"), K_("Xdt")], writes=["pY%d" % (h // 8)])
                        S.mark(5)
                        for g in range(2):
                            S.emit("pe", lambda e, g=g, cols=cols: e.matmul(pO[:], lhsT=Cz[:, g, cols], rhs=Hbf[:], start=True, stop=True),
                                   reads=["Cz", "Hbf"], writes=["pO"])
                            S.emit("dve", lambda e, g=g, s=s: e.tensor_tensor(out=yoffs[:, g * 512:(g + 1) * 512].rearrange("p (h d) -> p h d", h=8),
                                                                             in0=pO[:].rearrange("p (h d) -> p h d", h=8),
                                                                             in1=exps[s][:, g * 8:(g + 1) * 8].unsqueeze(2).to_broadcast([128, 8, 64]), op=ALU.mult),
                                   reads=["pO", K_("exps")], writes=["yoffs"])
                        S.mark(6)
                        for g in range(2):
                            S.emit("pe", lambda e, g=g, s=s: e.matmul(pD[g][:], lhsT=Btok[s][:], rhs=Xdec[s][:, g * 512:(g + 1) * 512], start=True, stop=True),
                                   reads=[K_("Btok"), K_("Xdec")], writes=["pD%d" % g])
                        for g in range(2):
                            rows = slice(g * 64, (g + 1) * 64)
                            S.emit("dve", lambda e, g=g, s=s, rows=rows: e.tensor_tensor(out=Hs[rows, :].rearrange("p (h d) -> p h d", h=8), in0=Hs[rows, :].rearrange("p (h d) -> p h d", h=8),
                                                                                       in1=exps[s][rows, 32 + g * 8:32 + (g + 1) * 8].unsqueeze(2).to_broadcast([64, 8, 64]), op=ALU.mult),
                                   reads=["Hs", K_("exps")], writes=["Hs"])
                            S.emit("dve", lambda e, g=g, rows=rows: e.tensor_tensor(out=Hs[rows, :], in0=pD[g][rows, :], in1=Hs[rows, :], op=ALU.add),
                                   reads=["Hs", "pD%d" % g], writes=["Hs"])
                            S.emit("act", lambda e, rows=rows: e.copy(out=Hbf[rows, :], in_=Hs[rows, :]), reads=["Hs"], writes=["Hbf"])
                        S.mark(7)

                        for b in range(2):
                            S.emit("dve", lambda e, b=b, s=s: e.tensor_tensor(out=ysb[s][:, b * 512:(b + 1) * 512], in0=pY[b][:], in1=yoffs[:, b * 512:(b + 1) * 512], op=ALU.add),
                                   reads=["pY%d" % b, "yoffs"], writes=[K_("ysb")])
                        S.emit("pool", lambda e, s=s: e.tensor_tensor(out=ysb[s][:], in0=ysb[s][:], in1=XD[s][:], op=ALU.add), reads=[K_("ysb"), K_("XD")], writes=[K_("ysb")])

                    def stage2a(c):
                        s = c % 2
                        K_ = lambda n, s=s: "%s%d" % (n, s)
                        dma("sp", zt[s][:], zs_s[c * 128:(c + 1) * 128, :], reads=[("zs", c)], writes=[K_("zt")])
                        S.emit("pool", lambda e, s=s: e.tensor_tensor(out=ysb[s][:], in0=ysb[s][:], in1=zt[s][:], op=ALU.mult), reads=[K_("ysb"), K_("zt")], writes=[K_("ysb")])
                        for g in range(2):
                            S.emit("act", lambda e, g=g, s=s: e.activation(out=junk2[:], in_=ysb[s][:, g * 512:(g + 1) * 512], func=AF.Square, accum_out=ss2[s][:, g:g + 1]),
                                   reads=[K_("ysb")], writes=["junk2", K_("ss2_")])
                        S.emit("act", lambda e, s=s: e.activation(out=rs2[s][:], in_=ss2[s][:], func=AF.Ln, scale=1.0 / 512, bias=EPS_T[:, 0:1]),
                               reads=[K_("ss2_"), "eps_t"], writes=[K_("rs2_")])
                        S.emit("act", lambda e, s=s: e.activation(out=rs2[s][:], in_=rs2[s][:], func=AF.Exp, scale=-0.5), reads=[K_("rs2_")], writes=[K_("rs2_")])
                        for g in range(2):
                            S.emit("dve", lambda e, g=g, s=s: e.scalar_tensor_tensor(out=yn[s][:, g * 512:(g + 1) * 512], in0=ysb[s][:, g * 512:(g + 1) * 512], scalar=rs2[s][:, g:g + 1],
                                                                                    in1=snwbc[:, g * 512:(g + 1) * 512], op0=ALU.mult, op1=ALU.mult),
                                   reads=[K_("ysb"), K_("rs2_"), "snwbc"], writes=[K_("yn")])

                    def stage2b(c):
                        s = c % 2
                        K_ = lambda n, s=s: "%s%d" % (n, s)
                        for k in range(KT):
                            S.emit("pe", lambda e, k=k, s=s: e.transpose(pX[:, k, :], yn[s][:, k * 128:(k + 1) * 128], ident_b[:]), reads=[K_("yn"), "ident_b"], writes=["pX"])
                        sgi = (c // 4) % 2
                        S.emit("act", lambda e, sgi=sgi, c=c: e.copy(out=stgT[sgi][:, :, (c % 4) * 128:(c % 4 + 1) * 128], in_=pX[:]), reads=["pX"], writes=["stgT%d" % sgi])
                        if c % 4 == 3:
                            n = c // 4
                            for k in range(KT):
                                dma("sp", ssdT_s[k * 128:(k + 1) * 128, n * 512:(n + 1) * 512], stgT[sgi][:, k, :], reads=["stgT%d" % sgi], writes=[("ssdT", n, k)])

                    NCk = 0 if raise_skip else NT
                    for i in range(NCk + 2):
                        if 2 <= i:
                            stage2a(i - 2)
                        if i < NCk:
                            stage1a(i)
                        if 1 <= i <= NCk:
                            stage1b(i - 1)
                        if 2 <= i:
                            stage2b(i - 2)
                    S.barrier()
                    S.flush()

        if upto >= 4:
            with ExitStack() as ph:
                vall = sbt(ph, "vall", [128, NT, 1040], BF16)
                gA = sbt(ph, "gA", [128, NT * 32], BF16)
                gM = sbt(ph, "gM", [128, NT * 32], BF16)
                gO = sbt(ph, "gO", [128, NT * 32], BF16)
                Sel = sbt(ph, "Sel", [128, 64], BF16)
                RD = [sbt(ph, "RD%d" % i, [65, 512], BF16) for i in range(2)]
                numS = [sbt(ph, "numS%d" % i, [64, 512], F32) for i in range(2)]
                attTh = [sbt(ph, "attTh%d" % i, [64, L], BF16) for i in range(2)]
                cmk = sbt(ph, "cmk", [128, 2048], BF16)
                qsb = [sbt(ph, "qsb_%d" % i, [128, L], BF16) for i in range(2)]
                ksb = [sbt(ph, "ksb_%d" % i, [128, L], BF16) for i in range(2)]
                kz = sbt(ph, "kz", [128, 2, L], BF16)
                qz = sbt(ph, "qz", [128, 2, L], BF16)
                negmw = sbt(ph, "negmw", [128, NT, 96], BF16)
                kms = sbt(ph, "kms", [128, 16], F32)
                kmz = sbt(ph, "kmz", [128, 2, 16], BF16)
                gate = sbt(ph, "gate", [128, NT, 2, 16], F32)
                top8 = sbt(ph, "top8", [128, NT, 2, 8], F32)
                alw = sbt(ph, "alw", [128, NT, 2, 16], F32)
                pTs = [sbt(ph, "pTs%d" % i, [128, 512], BF16) for i in range(8)]
                pS = [pst(ph, "pS%d" % i, [128, 512], F32) for i in range(6)]
                pOt = [pst(ph, "pOt%d" % i, [128, 512], F32) for i in range(2)]
                pG = pS[0]
                pBC = pS[1]
                dma("pool", gA[:], c_gA_d.partition_broadcast(128), writes=["gA"])
                dma("pool", gM[:], c_gM_d.partition_broadcast(128), writes=["gM"])
                dma("pool", gO[:], c_gO_d.partition_broadcast(128), writes=["gO"])
                S.emit("pool", lambda e: e.memset(Sel[:], 0.0), writes=["Sel"])
                S.emit("pool", lambda e: e.memset(Sel[64:65, :], 1.0), reads=["Sel"], writes=["Sel"])
                for i in range(2):
                    S.emit("pool", lambda e, i=i: e.memset(RD[i][:], 0.0), writes=["RD%d" % i])
                dma("pool", cmk[:], c_cm_d, writes=["cmk"])
                S.emit("pool", lambda e: e.memset(kz[:], 0.0), writes=[("kz", 0), ("kz", 1)])
                S.emit("pool", lambda e: e.memset(qz[:], 0.0), writes=[("qz", 0), ("qz", 1)])
                S.emit("pool", lambda e: e.memset(negmw[:], 0.0), writes=["negmw"])
                dma("pool", kz[64:80, 0, :], c_blkoh_d[:, 0:L], reads=[("kz", 0)], writes=[("kz", 0)])
                dma("pool", kz[0:16, 1, :], c_blkoh_d[:, 0:L], reads=[("kz", 1)], writes=[("kz", 1)])
                S.emit("pool", lambda e: e.memset(kmz[:], 0.0), writes=["kmz"])
                S.emit("pool", lambda e: e.memset(kms[:], 0.0), writes=["kms"])
                psn = [0]
                pt_i = 0
                po_i = 0
                for hp in range(8):
                    s = hp % 2
                    dma("sp", qsb[s][:], qT_s[hp * 128:(hp + 1) * 128, :], reads=[("qT", hp)], writes=["qsb_%d" % s])
                    dma("sp", ksb[s][:], kT_s[hp * 128:(hp + 1) * 128, :], reads=[("kT", hp)], writes=["ksb_%d" % s])
                    dma("sp", kz[0:64, 0, :], kT_s[hp * 128:hp * 128 + 64, :], reads=[("kT", hp), ("kz", 0)], writes=[("kz", 0)])
                    dma("sp", kz[64:128, 1, :], kT_s[hp * 128 + 64:(hp + 1) * 128, :], reads=[("kT", hp), ("kz", 1)], writes=[("kz", 1)])
                    dma("sp", qz[0:64, 0, :], qT_s[hp * 128:hp * 128 + 64, :], reads=[("qT", hp), ("qz", 0)], writes=[("qz", 0)])
                    dma("sp", qz[64:128, 1, :], qT_s[hp * 128 + 64:(hp + 1) * 128, :], reads=[("qT", hp), ("qz", 1)], writes=[("qz", 1)])
                    if hp == 0:
                        for t in range(NT):
                            dma("sp", vall[:, t, :], vaug_s[t * 128:(t + 1) * 128, :], reads=[("vaug", t)], writes=[("vall", t)])
                    S.emit("dve", lambda e, s=s: e.tensor_reduce(out=kms[:, 0:NB], in_=ksb[s][:].rearrange("p (n k) -> p n k", k=256), axis=AX.X, op=ALU.add),
                           reads=["ksb_%d" % s], writes=["kms"])
                    S.emit("dve", lambda e: e.tensor_scalar(out=kmz[0:64, 0, :], in0=kms[0:64, :], scalar1=1.0 / 256, scalar2=None, op0=ALU.mult), reads=["kms"], writes=["kmz"])
                    S.emit("dve", lambda e: e.tensor_scalar(out=kmz[64:128, 1, :], in0=kms[64:128, :], scalar1=1.0 / 256, scalar2=None, op0=ALU.mult), reads=["kms"], writes=["kmz"])
                    for t0 in range(0, NT, 16):
                        nt = min(16, NT - t0)
                        for t in range(t0, t0 + nt):
                            S.emit("pe", lambda e, s=s, t=t, t0=t0: e.matmul(pG[:, (t - t0) * 32:(t - t0 + 1) * 32], lhsT=qsb[s][:, t * 128:(t + 1) * 128],
                                                                            rhs=kmz[:].rearrange("p a n -> p (a n)"), start=True, stop=True),
                                   reads=["qsb_%d" % s, "kmz"], writes=["pS0"])
                        S.emit("dve", lambda e, t0=t0, nt=nt: e.tensor_copy(out=gate[:, t0:t0 + nt, :, :].rearrange("p t a n -> p (t a n)"), in_=pG[:, 0:nt * 32]),
                               reads=["pS0"], writes=["gate"])
                    S.emit("dve", lambda e: e.tensor_tensor(out=gate[:].rearrange("p t a n -> p (t a n)"), in0=gate[:].rearrange("p t a n -> p (t a n)"), in1=gA[:], op=ALU.add),
                           reads=["gate", "gA"], writes=["gate"])
                    for t in range(NT):
                        for a in range(2):
                            S.emit("dve", lambda e, t=t, a=a: e.max(out=top8[:, t, a, :], in_=gate[:, t, a, :]), reads=["gate"], writes=["top8"])
                    S.emit("dve", lambda e: e.tensor_tensor(out=alw[:].rearrange("p t a n -> p (t a) n"), in0=gate[:].rearrange("p t a n -> p (t a) n"),
                                                            in1=top8[:].rearrange("p t a k -> p (t a) k")[:, :, 2:3].to_broadcast([128, NT * 2, 16]), op=ALU.is_ge),
                           reads=["gate", "top8"], writes=["alw"])
                    S.emit("dve", lambda e: e.tensor_tensor(out=alw[:].rearrange("p t a n -> p (t a n)"), in0=alw[:].rearrange("p t a n -> p (t a n)"), in1=gM[:], op=ALU.mult),
                           reads=["alw", "gM"], writes=["alw"])
                    S.emit("dve", lambda e: e.tensor_tensor(out=alw[:].rearrange("p t a n -> p (t a n)"), in0=alw[:].rearrange("p t a n -> p (t a n)"), in1=gO[:], op=ALU.add),
                           reads=["alw", "gO"], writes=["alw"])
                    S.emit("dve", lambda e: e.tensor_scalar(out=negmw[:, :, 64:96], in0=alw[:].rearrange("p t a n -> p t (a n)"),
                                                            scalar1=-1.0, scalar2=BIG, op0=ALU.add, op1=ALU.mult), reads=["alw"], writes=["negmw"])
                    for a in range(2):
                        for t0 in range(0, NT, 4):
                            pgj = (t0 // 4) % 6
                            pgt, pgk = pS[pgj], "pS%d" % pgj
                            for t in range(t0, t0 + 4):
                                if a == 0:
                                    S.emit("pe", lambda e, t=t, t0=t0, pgt=pgt: e.matmul(pgt[0:80, (t - t0) * 128:(t - t0 + 1) * 128], lhsT=negmw[:, t, 0:80], rhs=ident_b[:], start=True, stop=True),
                                           reads=["negmw", "ident_b"], writes=[pgk])
                                else:
                                    S.emit("pe", lambda e, t=t, t0=t0, pgt=pgt: e.matmul(pgt[0:16, (t - t0) * 128:(t - t0 + 1) * 128], lhsT=negmw[:, t, 80:96], rhs=ident_b[:], start=True, stop=True),
                                           reads=["negmw", "ident_b"], writes=[pgk])
                            if a == 0:
                                S.emit("act", lambda e, t0=t0, pgt=pgt: e.copy(out=qz[64:80, 0, t0 * 128:(t0 + 4) * 128], in_=pgt[64:80, :]), reads=[pgk, ("qz", 0)], writes=[("qz", 0)])
                            else:
                                S.emit("dve", lambda e, t0=t0, pgt=pgt: e.tensor_copy(out=qz[0:16, 1, t0 * 128:(t0 + 4) * 128], in_=pgt[0:16, :]), reads=[pgk, ("qz", 1)], writes=[("qz", 1)])
                    for a in range(2):
                        h = hp * 2 + a
                        ah = h % 2
                        items = [(qc, kt) for qc in range(NCH) for kt in range(4 * (qc + 1))]

                        def emit_scores(qc, kt, slot, a=a):
                            r = kt - 4 * qc
                            c0 = max(r, 0) * 128
                            qcols = slice(qc * 512 + c0, (qc + 1) * 512)
                            S.emit("pe", lambda e: e.matmul(pS[slot][:, c0:512], lhsT=kz[:, a, kt * 128:(kt + 1) * 128], rhs=qz[:, a, qcols], start=True, stop=(r < 0)),
                                   reads=[("kz", a), ("qz", a)], writes=["pS%d" % slot])
                            if r >= 0:
                                S.emit("pe", lambda e: e.matmul(pS[slot][:, c0:512], lhsT=ident_b[:], rhs=cmk[:, r * 512 + c0:(r + 1) * 512], start=False, stop=True),
                                       reads=["ident_b", "cmk"], writes=["pS%d" % slot])

                        slots = {}
                        pending = []
                        AHEAD = 4
                        for ii in range(min(AHEAD, len(items))):
                            slots[ii] = psn[0] % 6
                            psn[0] += 1
                            emit_scores(items[ii][0], items[ii][1], slots[ii])
                        for ii, (qc, kt) in enumerate(items):
                            if ii + AHEAD < len(items):
                                slots[ii + AHEAD] = psn[0] % 6
                                psn[0] += 1
                                emit_scores(items[ii + AHEAD][0], items[ii + AHEAD][1], slots[ii + AHEAD])
                            slot = slots[ii]
                            pt = pt_i % 8
                            pt_i += 1
                            r = kt - 4 * qc
                            c0 = max(r, 0) * 128
                            S.emit("act", lambda e, slot=slot, pt=pt, c0=c0: e.activation(out=pTs[pt][:, c0:512], in_=pS[slot][:, c0:512], func=AF.Exp, scale=0.125),
                                   reads=["pS%d" % slot], writes=["pTs%d" % pt])
                            if kt == 0:
                                po = po_i % 2
                                po_i += 1
                            S.emit("pe", lambda e, pt=pt, po=po, kt=kt, h=h, qc=qc, c0=c0: e.matmul(
                                pOt[po][0:65, c0:512], lhsT=vall[:, kt, h * 65:(h + 1) * 65], rhs=pTs[pt][:, c0:512],
                                start=(kt == 0), stop=(kt == 4 * qc + 3)),
                                reads=["pTs%d" % pt, ("vall", kt)], writes=["pOt%d" % po])
                            def norm_tail(po=po, qc=qc, ah=ah):
                                bs = psn[0] % 6
                                psn[0] += 1
                                S.emit("pe", lambda e: e.matmul(pS[bs][0:64, :], lhsT=Sel[0:65, :], rhs=RD[po][:], start=True, stop=True),
                                       reads=["Sel", "RD%d" % po], writes=["pS%d" % bs])
                                S.emit("dve", lambda e: e.tensor_tensor(out=attTh[ah][:, qc * 512:(qc + 1) * 512], in0=pS[bs][0:64, :], in1=numS[po][:], op=ALU.mult),
                                       reads=["pS%d" % bs, "numS%d" % po], writes=["attTh%d" % ah])
                            for pn in list(pending):
                                pn[0] -= 1
                                if pn[0] <= 0:
                                    pn[1]()
                                    pending.remove(pn)
                            if kt == 4 * qc + 3:
                                S.emit("dve", lambda e, po=po: e.reciprocal(out=RD[po][64:65, :], in_=pOt[po][64:65, :]), reads=["pOt%d" % po], writes=["RD%d" % po])
                                S.emit("act", lambda e, po=po: e.copy(out=numS[po][:], in_=pOt[po][0:64, :]), reads=["pOt%d" % po], writes=["numS%d" % po])
                                pending.append([7, norm_tail])
                        for pn in pending:
                            pn[1]()
                        pending = []
                        dma("sp", attT_s[h * 64:(h + 1) * 64, :], attTh[ah][:], reads=["attTh%d" % ah], writes=[("attT", h)])
                S.barrier()
                S.flush()

        if upto >= 5:
            h2T_s = dscr("h2T_s", [D, L], BF16)
            with ExitStack() as ph:
                Wso = sbt(ph, "Wso", [128, KT, D], BF16)
                Wao = sbt(ph, "Wao", [128, KT, D], BF16)
                Wo = sbt(ph, "Wo", [128, KT, D], BF16)
                nw2bc = sbt(ph, "nw2bc", [128, D], F32)
                ssc = [sbt(ph, "ssc%d" % i, [128, KT, 512], BF16) for i in range(2)]
                atc = [sbt(ph, "atc%d" % i, [128, KT, 512], BF16) for i in range(2)]
                sgs = [sbt(ph, "sgs%d" % i, [128, KT, 512], BF16) for i in range(2)]
                sga = [sbt(ph, "sga%d" % i, [128, KT, 512], BF16) for i in range(2)]
                mT = [sbt(ph, "mT%d" % i, [128, KT, 512], BF16) for i in range(2)]
                m1 = [sbt(ph, "m1_%d" % i, [128, 512], F32) for i in range(2)]
                m2 = [sbt(ph, "m2_%d" % i, [128, 512], F32) for i in range(2)]
                xin2 = [sbt(ph, "xin2_%d" % i, [128, D], F32) for i in range(2)]
                x1t = [sbt(ph, "x1t%d" % i, [128, D], F32) for i in range(2)]
                hn2 = [sbt(ph, "hn2_%d" % i, [128, D], BF16) for i in range(2)]
                junk3 = sbt(ph, "junk3", [128, D], BF16)
                ssd_ = [sbt(ph, "ssD%d" % i, [128, 1], F32) for i in range(2)]
                rsd_ = [sbt(ph, "rsD%d" % i, [128, 1], F32) for i in range(2)]
                h2st = [sbt(ph, "h2st%d" % i, [128, KT, 512], BF16) for i in range(2)]
                pA = [pst(ph, "pA%d" % i, [128, 512], F32) for i in range(2)]
                pB = [pst(ph, "pB%d" % i, [128, 512], F32) for i in range(2)]
                pX1 = [pst(ph, "pX1_%d" % i, [128, 512], F32) for i in range(2)]
                pTd = pst(ph, "pTd", [128, KT, 128], BF16)
                for cb in range(0, KT, 2):
                    csl = slice(cb * 128, (cb + 2) * 128)
                    dma("pool", Wso[:, :, csl], wso_d[:, csl].rearrange("(k p) c -> p k c", p=128), writes=[("Wso", cb), ("Wso", cb + 1)])
                    dma("pool", Wao[:, :, csl], wao_d[:, csl].rearrange("(k p) c -> p k c", p=128), writes=[("Wao", cb), ("Wao", cb + 1)])
                for hb in range(2):
                    hsl = slice(hb * 512, (hb + 1) * 512)
                    dma("pool", Wo[:, :, hsl], wo_d[:, hsl].rearrange("(k p) c -> p k c", p=128), writes=[("Wo", hb)])
                dma("sp", nw2bc[:], nw2_d.partition_broadcast(128), writes=["nw2bc"])
                ab_i = 0
                x_i = 0
                deferredD = []
                for n in range(NCH):
                    s = n % 2
                    cs = slice(n * 512, (n + 1) * 512)
                    dma("sp", ssc[s][:], ssdT_s[:, cs].rearrange("(k p) t -> p k t", p=128), writes=["ssc%d" % s])
                    dma("sp", atc[s][:], attT_s[:, cs].rearrange("(k p) t -> p k t", p=128), writes=["atc%d" % s])
                    dma("sp", sgs[s][:], sgT_s[0:D, cs].rearrange("(k p) t -> p k t", p=128), writes=["sgs%d" % s])
                    dma("sp", sga[s][:], sgT_s[D:2 * D, cs].rearrange("(k p) t -> p k t", p=128), writes=["sga%d" % s])
                    for c in range(KT):
                        i = ab_i % 2
                        ab_i += 1
                        for k in range(KT):
                            S.emit("pe", lambda e, i=i, k=k, c=c, s=s: e.matmul(pA[i][:], lhsT=Wso[:, k, c * 128:(c + 1) * 128], rhs=ssc[s][:, k, :], start=(k == 0), stop=(k == KT - 1)),
                                   reads=[("Wso", c), "ssc%d" % s], writes=["pA%d" % i])
                        for k in range(KT):
                            S.emit("pe", lambda e, i=i, k=k, c=c, s=s: e.matmul(pB[i][:], lhsT=Wao[:, k, c * 128:(c + 1) * 128], rhs=atc[s][:, k, :], start=(k == 0), stop=(k == KT - 1)),
                                   reads=[("Wao", c), "atc%d" % s], writes=["pB%d" % i])
                        S.emit("dve", lambda e, i=i, c=c, s=s: e.tensor_tensor(out=m1[i][:], in0=pA[i][:], in1=sgs[s][:, c, :], op=ALU.mult),
                               reads=["pA%d" % i, "sgs%d" % s], writes=["m1_%d" % i])
                        S.emit("dve", lambda e, i=i, c=c, s=s: e.tensor_tensor(out=m2[i][:], in0=pB[i][:], in1=sga[s][:, c, :], op=ALU.mult),
                               reads=["pB%d" % i, "sga%d" % s], writes=["m2_%d" % i])
                        S.emit("pool", lambda e, i=i, c=c, s=s: e.tensor_tensor(out=mT[s][:, c, :], in0=m1[i][:], in1=m2[i][:], op=ALU.add),
                               reads=["m1_%d" % i, "m2_%d" % i], writes=["mT%d" % s])
                    def d_s1(n, j, s):
                        t = n * 4 + j
                        xs = t % 2
                        dma("sp", xin2[xs][:], x_d[t * 128:(t + 1) * 128, :], writes=["xin2_%d" % xs])
                        for hf in range(2):
                            for k in range(KT):
                                S.emit("pe", lambda e, hf=hf, k=k, j=j, s=s: e.matmul(pX1[hf][:], lhsT=mT[s][:, k, j * 128:(j + 1) * 128], rhs=Wo[:, k, hf * 512:(hf + 1) * 512],
                                                                                     start=(k == 0), stop=(k == KT - 1)),
                                       reads=[("Wo", hf), "mT%d" % s], writes=["pX1_%d" % hf])
                            S.emit("dve", lambda e, hf=hf, xs=xs: e.tensor_tensor(out=x1t[xs][:, hf * 512:(hf + 1) * 512], in0=pX1[hf][:], in1=xin2[xs][:, hf * 512:(hf + 1) * 512], op=ALU.add),
                                   reads=["pX1_%d" % hf, "xin2_%d" % xs], writes=["x1t%d" % xs])
                        dma("sp", x1_s[t * 128:(t + 1) * 128, :], x1t[xs][:], reads=["x1t%d" % xs], writes=[("x1s", t)])
                        S.emit("act", lambda e, xs=xs: e.activation(out=junk3[:], in_=x1t[xs][:], func=AF.Square, accum_out=ssd_[xs][:]),
                               reads=["x1t%d" % xs], writes=["junk3", "ssD%d" % xs])
                        rstd_from_ss(ssd_[xs][:], rsd_[xs][:], D, ["ssD%d" % xs], ["rsD%d" % xs])
                        S.emit("dve", lambda e, xs=xs: e.scalar_tensor_tensor(out=hn2[xs][:], in0=x1t[xs][:], scalar=rsd_[xs][:, 0:1], in1=nw2bc[:], op0=ALU.mult, op1=ALU.mult),
                               reads=["x1t%d" % xs, "rsD%d" % xs, "nw2bc"], writes=["hn2_%d" % xs])

                    def d_s2(n, j, s):
                        t = n * 4 + j
                        xs = t % 2
                        for k in range(KT):
                            S.emit("pe", lambda e, k=k, xs=xs: e.transpose(pTd[:, k, :], hn2[xs][:, k * 128:(k + 1) * 128], ident_b[:]), reads=["hn2_%d" % xs, "ident_b"], writes=["pTd"])
                        S.emit("act", lambda e, s=s, j=j: e.copy(out=h2st[s][:, :, j * 128:(j + 1) * 128], in_=pTd[:]), reads=["pTd"], writes=["h2st%d" % s])

                    def d_store(n, s):
                        cs_ = slice(n * 512, (n + 1) * 512)
                        for k in range(KT):
                            dma("sp", h2T_s[k * 128:(k + 1) * 128, cs_], h2st[s][:, k, :], reads=["h2st%d" % s], writes=[("h2T", n, k)])

                    for fn in deferredD:
                        fn()
                    deferredD = []
                    d_s1(n, 0, s)
                    d_s1(n, 1, s)
                    d_s2(n, 0, s)
                    d_s1(n, 2, s)
                    d_s2(n, 1, s)
                    d_s1(n, 3, s)
                    d_s2(n, 2, s)
                    deferredD = [lambda n=n, s=s: d_s2(n, 3, s), lambda n=n, s=s: d_store(n, s)]
                for fn in deferredD:
                    fn()
                S.barrier()
                S.flush()

        if upto >= 6:
            PTOK = min(L, 2048)
            for part in range(L // PTOK):
                tok0 = part * PTOK
                PT_ = PTOK // 128
                PC_ = PTOK // 512
                with ExitStack() as pp_:
                    acc = sbt(pp_, "acc", [128, PT_, D], F32)
                    Wpg = sbt(pp_, "Wpg", [128, KT, D], BF16)
                    Wple = sbt(pp_, "Wple", [128, 2, D], BF16)
                    nw3bc = sbt(pp_, "nw3bc", [128, D], F32)
                    nwfbc = sbt(pp_, "nwfbc", [128, D], F32)
                    with ExitStack() as ph:
                        h2 = sbt(ph, "h2", [128, KT, PTOK], BF16)
                        Wrt = sbt(ph, "Wrt", [128, KT, 36], BF16)
                        brt = sbt(ph, "brt", [128, 36], F32)
                        comb = sbt(ph, "comb", [128, PT_, 32], F32)
                        lgB = sbt(ph, "lgB", [128, 8, 36], F32)
                        gmxB = sbt(ph, "gmxB", [128, 8], F32)
                        gohB = sbt(ph, "gohB", [128, 8, 4], F32)
                        gexB = sbt(ph, "gexB", [128, 8, 4], F32)
                        gwB = sbt(ph, "gwB", [128, 8], F32)
                        eltB = sbt(ph, "eltB", [128, 8, 4, 8], F32)
                        elsB = sbt(ph, "elsB", [128, 8, 8], F32)
                        t8B = sbt(ph, "t8B", [128, 8, 8], F32)
                        scB = sbt(ph, "scB", [128, 7, 8], F32)
                        eaB = sbt(ph, "eaB", [128, 8, 8], F32)
                        ebB = sbt(ph, "ebB", [128, 8, 8], F32)
                        lg = sbt(ph, "lg", [128, 36], F32)
                        gmx = sbt(ph, "gmx", [128, 1], F32)
                        ngmx = sbt(ph, "ngmx", [128, 1], F32)
                        goh = sbt(ph, "goh", [128, 4], F32)
                        gex = sbt(ph, "gex", [128, 4], F32)
                        gsum = sbt(ph, "gsum", [128, 1], F32)
                        gw = sbt(ph, "gw", [128, 1], F32)
                        elt = sbt(ph, "elt", [128, 4, 8], F32)
                        els = sbt(ph, "els", [128, 8], F32)
                        t8 = sbt(ph, "t8", [128, 8], F32)
                        sc = sbt(ph, "sc", [128, 8], F32)
                        ea = sbt(ph, "ea", [128, 8], F32)
                        eb = sbt(ph, "eb", [128, 8], F32)
                        Wg = [sbt(ph, "Wg%d" % i, [128, KT, 256], BF16) for i in range(2)]
                        Wu = [sbt(ph, "Wu%d" % i, [128, KT, 256], BF16) for i in range(2)]
                        Wd = [sbt(ph, "Wd%d" % i, [128, 2, D], BF16) for i in range(2)]
                        hid = [sbt(ph, "hid%d" % i, [128, 2, PTOK], BF16) for i in range(2)]
                        sgt = [sbt(ph, "sgt%d" % i, [128, 512], F32) for i in range(2)]
                        etmp = [sbt(ph, "etmp%d" % i, [128, 512], F32) for i in range(2)]
                        pGt = [pst(ph, "pGt%d" % i, [128, 512], F32) for i in range(2)]
                        pUt = [pst(ph, "pUt%d" % i, [128, 512], F32) for i in range(2)]
                        pDn = [pst(ph, "pDn%d" % i, [128, 512], F32) for i in range(4)]
                        pR = pDn[3]
                        for k in range(KT):
                            dma("sp", h2[:, k, :], h2T_s[k * 128:(k + 1) * 128, tok0:tok0 + PTOK], writes=[("h2", k)])
                        dma("pool", Wrt[:], wrt_d.rearrange("(k p) c -> p k c", p=128), writes=["Wrt"])
                        dma("sp", brt[:], brt_d.partition_broadcast(128), writes=["brt"])
                        for t in range(PT_):
                            dma("sp", acc[:, t, :], x1_s[tok0 + t * 128:tok0 + (t + 1) * 128, :], reads=[("x1s", tok0 // 128 + t)], writes=[("acc", t)])
                        h2keys = [("h2", k) for k in range(KT)]
                        RB = 8
                        for t0 in range(0, PT_, RB):
                            TB = [128, RB, 8]
                            for tt in range(RB):
                                t = t0 + tt
                                for k in range(KT):
                                    S.emit("pe", lambda e, k=k, t=t, tt=tt: e.matmul(pR[:, tt * 36:(tt + 1) * 36], lhsT=h2[:, k, t * 128:(t + 1) * 128], rhs=Wrt[:, k, :],
                                                                                    start=(k == 0 and tt == 0), stop=(k == KT - 1)),
                                           reads=h2keys + ["Wrt"], writes=["pDn3"])
                            prv = pR[:, 0:RB * 36].rearrange("p (t c) -> p t c", t=RB)
                            S.emit("dve", lambda e, prv=prv: e.tensor_tensor(out=lgB[:], in0=prv, in1=brt[:].unsqueeze(1).to_broadcast([128, RB, 36]), op=ALU.add),
                                   reads=["pDn3", "brt"], writes=["lgB"])
                            S.emit("dve", lambda e: e.tensor_reduce(out=gmxB[:], in_=lgB[:, :, 0:4], axis=AX.X, op=ALU.max), reads=["lgB"], writes=["gmxB"])
                            S.emit("dve", lambda e: e.tensor_tensor(out=gohB[:], in0=lgB[:, :, 0:4], in1=gmxB[:].unsqueeze(2).to_broadcast([128, RB, 4]), op=ALU.is_ge),
                                   reads=["lgB", "gmxB"], writes=["gohB"])
                            S.emit("dve", lambda e: e.tensor_tensor(out=gexB[:], in0=lgB[:, :, 0:4], in1=gmxB[:].unsqueeze(2).to_broadcast([128, RB, 4]), op=ALU.subtract),
                                   reads=["lgB", "gmxB"], writes=["gexB"])
                            S.emit("act", lambda e: e.activation(out=gexB[:], in_=gexB[:], func=AF.Exp), reads=["gexB"], writes=["gexB"])
                            S.emit("dve", lambda e: e.tensor_reduce(out=gwB[:], in_=gexB[:], axis=AX.X, op=ALU.add), reads=["gexB"], writes=["gwB"])
                            S.emit("dve", lambda e: e.reciprocal(out=gwB[:], in_=gwB[:]), reads=["gwB"], writes=["gwB"])
                            S.emit("dve", lambda e: e.tensor_tensor(out=eltB[:], in0=lgB[:, :, 4:36].rearrange("p t (g e) -> p t g e", g=4),
                                                                    in1=gohB[:].unsqueeze(3).to_broadcast([128, RB, 4, 8]), op=ALU.mult),
                                   reads=["lgB", "gohB"], writes=["eltB"])
                            S.emit("dve", lambda e: e.tensor_reduce(out=elsB[:], in_=eltB[:].rearrange("p t g e -> p t e g"), axis=AX.X, op=ALU.add), reads=["eltB"], writes=["elsB"])
                            for tt in range(RB):
                                S.emit("dve", lambda e, tt=tt: e.max(out=t8B[:, tt, :], in_=elsB[:, tt, :]), reads=["elsB"], writes=["t8B"])
                            v1b = lambda: t8B[:, :, 0:1].to_broadcast(TB)
                            v2b = lambda: t8B[:, :, 1:2].to_broadcast(TB)
                            S.emit("dve", lambda e: e.tensor_tensor(out=scB[:, 0, :], in0=t8B[:, :, 1], in1=t8B[:, :, 0], op=ALU.subtract), reads=["t8B"], writes=["scB"])
                            S.emit("act", lambda e: e.activation(out=scB[:, 1, :], in_=scB[:, 0, :], func=AF.Exp), reads=["scB"], writes=["scB"])
                            S.emit("dve", lambda e: e.tensor_scalar(out=scB[:, 2, :], in0=scB[:, 1, :], scalar1=1.0, scalar2=None, op0=ALU.add), reads=["scB"], writes=["scB"])
                            S.emit("dve", lambda e: e.reciprocal(out=scB[:, 3, :], in_=scB[:, 2, :]), reads=["scB"], writes=["scB"])
                            S.emit("dve", lambda e: e.tensor_tensor(out=scB[:, 4, :], in0=scB[:, 3, :], in1=gwB[:], op=ALU.mult), reads=["scB", "gwB"], writes=["scB"])
                            S.emit("dve", lambda e: e.tensor_tensor(out=scB[:, 5, :], in0=gwB[:], in1=scB[:, 4, :], op=ALU.subtract), reads=["scB", "gwB"], writes=["scB"])
                            S.emit("dve", lambda e: e.tensor_tensor(out=scB[:, 6, :], in0=scB[:, 4, :], in1=scB[:, 5, :], op=ALU.subtract), reads=["scB"], writes=["scB"])
                            S.emit("dve", lambda e: e.tensor_tensor(out=eaB[:], in0=elsB[:], in1=v1b(), op=ALU.is_ge), reads=["elsB", "t8B"], writes=["eaB"])
                            S.emit("dve", lambda e: e.tensor_tensor(out=eaB[:], in0=eaB[:], in1=scB[:, 6, :].unsqueeze(2).to_broadcast(TB), op=ALU.mult), reads=["eaB", "scB"], writes=["eaB"])
                            S.emit("dve", lambda e: e.tensor_tensor(out=ebB[:], in0=elsB[:], in1=v2b(), op=ALU.is_ge), reads=["elsB", "t8B"], writes=["ebB"])
                            S.emit("dve", lambda e: e.tensor_tensor(out=ebB[:], in0=ebB[:], in1=scB[:, 5, :].unsqueeze(2).to_broadcast(TB), op=ALU.mult), reads=["ebB", "scB"], writes=["ebB"])
                            S.emit("dve", lambda e: e.tensor_tensor(out=eaB[:], in0=eaB[:], in1=ebB[:], op=ALU.add), reads=["eaB", "ebB"], writes=["eaB"])
                            for g in range(4):
                                S.emit("dve", lambda e, g=g, t0=t0: e.tensor_tensor(out=comb[:, t0:t0 + RB, g * 8:(g + 1) * 8], in0=eaB[:], in1=gohB[:, :, g:g + 1].to_broadcast(TB), op=ALU.mult),
                                       reads=["eaB", "gohB"], writes=["comb"])
                        cnt = {"gu": 0, "dn": 0}

                        def emit_gu(ex, n, f):
                            s = ex % 2
                            cs = slice(n * 512, (n + 1) * 512)
                            i = cnt["gu"] % 2
                            cnt["gu"] += 1
                            for k in range(KT):
                                S.emit("pe", lambda e, i=i, k=k, f=f, s=s, cs=cs: e.matmul(pGt[i][:], lhsT=Wg[s][:, k, f * 128:(f + 1) * 128], rhs=h2[:, k, cs], start=(k == 0), stop=(k == KT - 1)),
                                       reads=["Wg%d" % s] + h2keys, writes=["pGt%d" % i])
                            for k in range(KT):
                                S.emit("pe", lambda e, i=i, k=k, f=f, s=s, cs=cs: e.matmul(pUt[i][:], lhsT=Wu[s][:, k, f * 128:(f + 1) * 128], rhs=h2[:, k, cs], start=(k == 0), stop=(k == KT - 1)),
                                       reads=["Wu%d" % s] + h2keys, writes=["pUt%d" % i])
                            S.emit("act", lambda e, i=i: e.activation(out=sgt[i][:], in_=pGt[i][:], func=AF.Silu), reads=["pGt%d" % i], writes=["sgt%d" % i])
                            S.emit("dve", lambda e, i=i, f=f, s=s, cs=cs: e.tensor_tensor(out=hid[s][:, f, cs], in0=pUt[i][:], in1=sgt[i][:], op=ALU.mult),
                                   reads=["pUt%d" % i, "sgt%d" % i], writes=[("hid%d" % s, f, n)])

                        def emit_dn(ex, t, hf):
                            s = ex % 2
                            n = t // 4
                            i = cnt["dn"] % 4
                            cnt["dn"] += 1
                            for f in range(2):
                                S.emit("pe", lambda e, i=i, f=f, t=t, hf=hf, s=s: e.matmul(pDn[i][:], lhsT=hid[s][:, f, t * 128:(t + 1) * 128], rhs=Wd[s][:, f, hf * 512:(hf + 1) * 512],
                                                                                       start=(f == 0), stop=(f == 1)),
                                       reads=[("hid%d" % s, f, n), "Wd%d" % s], writes=["pDn%d" % i])
                            S.emit("dve", lambda e, i=i, t=t, hf=hf, ex=ex: e.scalar_tensor_tensor(out=acc[:, t, hf * 512:(hf + 1) * 512], in0=pDn[i][:], scalar=comb[:, t, ex:ex + 1],
                                                                                               in1=acc[:, t, hf * 512:(hf + 1) * 512], op0=ALU.mult, op1=ALU.add),
                                   reads=["pDn%d" % i, "comb", ("acc", t)], writes=[("acc", t)])

                        dma("sp", nw3bc[:], nw3_d.partition_broadcast(128), writes=["nw3bc"])
                        dma("sp", nwfbc[:], nwf_d.partition_broadcast(128), writes=["nwfbc"])
                        for ex in range(33):
                            if ex == 2:
                                dma("pool", Wpg[:], wpg_d.rearrange("(k p) c -> p k c", p=128), writes=["Wpg"])
                                dma("pool", Wple[:], wple_d.rearrange("(j p) c -> p j c", p=128), writes=["Wple"])
                            if ex < 32:
                                s = ex % 2
                                dma("pool", Wg[s][:], wg_d[ex].rearrange("(k p) f -> p k f", p=128), writes=["Wg%d" % s])
                                dma("pool", Wu[s][:], wu_d[ex].rearrange("(k p) f -> p k f", p=128), writes=["Wu%d" % s])
                                dma("pool", Wd[s][:], wd_d[ex].rearrange("(j p) c -> p j c", p=128), writes=["Wd%d" % s])
                            for n in range(PC_):
                                for f in range(2):
                                    if ex < 32:
                                        emit_gu(ex, n, f)
                                    if ex >= 1:
                                        for t in (4 * n + 2 * f, 4 * n + 2 * f + 1):
                                            for hf in range(2):
                                                emit_dn(ex - 1, t, hf)
                        S.barrier()
                        S.flush()
                    with ExitStack() as ph:
                        junk4 = sbt(ph, "junk4", [128, D], BF16)
                        ss3 = [sbt(ph, "ss3_%d" % i, [128, 1], F32) for i in range(2)]
                        rs3 = [sbt(ph, "rs3_%d" % i, [128, 1], F32) for i in range(2)]
                        ssf = [sbt(ph, "ssf_%d" % i, [128, 1], F32) for i in range(2)]
                        rsf = [sbt(ph, "rsf_%d" % i, [128, 1], F32) for i in range(2)]
                        h3 = [sbt(ph, "h3_%d" % i, [128, D], BF16) for i in range(2)]
                        h3T = [sbt(ph, "h3T%d" % i, [128, KT, 128], BF16) for i in range(2)]
                        pin = [sbt(ph, "pin%d" % i, [128, 256], F32) for i in range(2)]
                        pbf = [sbt(ph, "pbf%d" % i, [128, 256], BF16) for i in range(2)]
                        ppT = [sbt(ph, "ppT%d" % i, [128, 2, 128], BF16) for i in range(2)]
                        sgp = [sbt(ph, "sgp%d" % i, [128, D], F32) for i in range(2)]
                        x3 = [sbt(ph, "x3_%d" % i, [128, D], F32) for i in range(2)]
                        ot = [sbt(ph, "ot%d" % i, [128, D], F32) for i in range(2)]
                        pT3 = pst(ph, "pT3", [128, KT, 128], BF16)
                        pTp = pst(ph, "pTp", [128, KT, 128], BF16)
                        pGp = [pst(ph, "pGp%d" % i, [128, 512], F32) for i in range(2)]
                        pPp = [pst(ph, "pPp%d" % i, [128, 512], F32) for i in range(2)]
                        def p_s1(t):
                            s = t % 2
                            K_ = lambda nm, s=s: "%s%d" % (nm, s)
                            rows = slice(tok0 + t * 128, tok0 + (t + 1) * 128)
                            dma("sp", pin[s][:], p_d[rows, :], writes=[K_("pin")])
                            S.emit("act", lambda e, s=s, t=t: e.activation(out=junk4[:], in_=acc[:, t, :], func=AF.Square, accum_out=ss3[s][:]), reads=[("acc", t)], writes=["junk4", K_("ss3_")])
                            rstd_pow(ss3[s][:], rs3[s][:], D, [K_("ss3_")], [K_("rs3_")])
                            S.emit("dve", lambda e, s=s, t=t: e.scalar_tensor_tensor(out=h3[s][:], in0=acc[:, t, :], scalar=rs3[s][:, 0:1], in1=nw3bc[:], op0=ALU.mult, op1=ALU.mult),
                                   reads=[("acc", t), K_("rs3_"), "nw3bc"], writes=[K_("h3_")])
                            S.emit("pool", lambda e, s=s: e.tensor_copy(out=pbf[s][:], in_=pin[s][:]), reads=[K_("pin")], writes=[K_("pbf")])

                        def p_s2(t):
                            s = t % 2
                            K_ = lambda nm, s=s: "%s%d" % (nm, s)
                            for k in range(KT):
                                S.emit("pe", lambda e, k=k, s=s: e.transpose(pT3[:, k, :], h3[s][:, k * 128:(k + 1) * 128], ident_b[:]), reads=[K_("h3_"), "ident_b"], writes=["pT3"])
                            S.emit("act", lambda e, s=s: e.copy(out=h3T[s][:], in_=pT3[:]), reads=["pT3"], writes=[K_("h3T")])
                            for j in range(2):
                                S.emit("pe", lambda e, j=j, s=s: e.transpose(pTp[:, j, :], pbf[s][:, j * 128:(j + 1) * 128], ident_b[:]), reads=[K_("pbf"), "ident_b"], writes=["pTp"])
                            S.emit("act", lambda e, s=s: e.copy(out=ppT[s][:], in_=pTp[:, 0:2, :]), reads=["pTp"], writes=[K_("ppT")])
                            for hf in range(2):
                                hs = slice(hf * 512, (hf + 1) * 512)
                                for k in range(KT):
                                    S.emit("pe", lambda e, k=k, hf=hf, s=s, hs=hs: e.matmul(pGp[hf][:], lhsT=h3T[s][:, k, :], rhs=Wpg[:, k, hs], start=(k == 0), stop=(k == KT - 1)),
                                           reads=[K_("h3T"), "Wpg"], writes=["pGp%d" % hf])
                                for j in range(2):
                                    S.emit("pe", lambda e, j=j, hf=hf, s=s, hs=hs: e.matmul(pPp[hf][:], lhsT=ppT[s][:, j, :], rhs=Wple[:, j, hs], start=(j == 0), stop=(j == 1)),
                                           reads=[K_("ppT"), "Wple"], writes=["pPp%d" % hf])
                                S.emit("act", lambda e, hf=hf, s=s, hs=hs: e.activation(out=sgp[s][:, hs], in_=pGp[hf][:], func=AF.Sigmoid), reads=["pGp%d" % hf], writes=[K_("sgp")])
                                S.emit("dve", lambda e, hf=hf, s=s, hs=hs: e.tensor_tensor(out=x3[s][:, hs], in0=pPp[hf][:], in1=sgp[s][:, hs], op=ALU.mult),
                                       reads=["pPp%d" % hf, K_("sgp")], writes=[K_("x3_")])
                            S.emit("pool", lambda e, s=s, t=t: e.tensor_tensor(out=x3[s][:], in0=x3[s][:], in1=acc[:, t, :], op=ALU.add), reads=[K_("x3_"), ("acc", t)], writes=[K_("x3_")])

                        def p_s3(t):
                            s = t % 2
                            K_ = lambda nm, s=s: "%s%d" % (nm, s)
                            rows = slice(tok0 + t * 128, tok0 + (t + 1) * 128)
                            S.emit("act", lambda e, s=s: e.activation(out=junk4[:], in_=x3[s][:], func=AF.Square, accum_out=ssf[s][:]), reads=[K_("x3_")], writes=["junk4", K_("ssf_")])
                            rstd_pow(ssf[s][:], rsf[s][:], D, [K_("ssf_")], [K_("rsf_")])
                            S.emit("dve", lambda e, s=s: e.scalar_tensor_tensor(out=ot[s][:], in0=x3[s][:], scalar=rsf[s][:, 0:1], in1=nwfbc[:], op0=ALU.mult, op1=ALU.mult),
                                   reads=[K_("x3_"), K_("rsf_"), "nwfbc"], writes=[K_("ot")])
                            dma("sp", out_d[rows, :], ot[s][:], reads=[K_("ot")], writes=[("out", tok0 // 128 + t)])

                        for t in range(PT_ + 2):
                            if t < PT_:
                                p_s1(t)
                            if 1 <= t <= PT_:
                                p_s2(t - 1)
                            if t >= 2:
                                p_s3(t - 2)
                        S.barrier()
                        S.flush()
    return nc, dbg_out


def make_in_maps(inputs, L, cores):
    c = host_consts(L)
    f = lambda a: np.ascontiguousarray(a, dtype=np.float32)
    cw = inputs["conv_w"][0]
    cw_l = np.ascontiguousarray(cw.reshape(4, 10, 128).transpose(2, 1, 0).reshape(128, 40))
    cb_l = np.ascontiguousarray(inputs["conv_b"][0].reshape(10, 128).T)
    shared = {
        "w_in": f(inputs["w_in"][0]),
        "attn_norm_w": f(inputs["attn_norm_w"][0:1]),
        "conv_w": f(cw_l), "conv_b": f(cb_l),
        "dt_bias": f(inputs["dt_bias"][0:1]), "a_log": f(inputs["a_log"][0:1]), "d_skip": f(inputs["d_skip"][0:1]),
        "ssd_norm_w": f(inputs["ssd_norm_w"][0:1]),
        "w_ssd_out": f(inputs["w_ssd_out"][0]), "w_attn_out": f(inputs["w_attn_out"][0]), "w_out": f(inputs["w_out"][0]),
        "moe_norm_w": f(inputs["moe_norm_w"][0:1]),
        "w_rt": f(np.concatenate([inputs["w_router_group"][0], inputs["w_router_expert"][0]], axis=1)),
        "b_rt": f(np.concatenate([inputs["b_router_group"][0], inputs["b_router_expert"][0]])[None, :]),
        "w_exp_gate": f(inputs["w_exp_gate"][0].reshape(32, D, 256)),
        "w_exp_up": f(inputs["w_exp_up"][0].reshape(32, D, 256)),
        "w_exp_down": f(inputs["w_exp_down"][0].reshape(32, 256, D)),
        "ple_norm_w": f(inputs["ple_norm_w"][0:1]),
        "w_ple": f(inputs["w_ple"][0]), "w_ple_gate": f(inputs["w_ple_gate"][0]),
        "final_norm_w": f(inputs["final_norm_w"][None, :]),
    }
    for k, v in c.items():
        shared["c_" + k] = v
    maps = []
    for b in cores:
        m = dict(shared)
        m["x"] = f(inputs["x"][b, :L])
        m["p"] = f(inputs["p"][0, b, :L])
        m["pos"] = np.ascontiguousarray(inputs["positions"][b:b + 1, :L].astype(np.int32))
        maps.append(m)
    return maps


def kernel(**inputs):
    L = inputs["x"].shape[1]
    nc, _ = build(L)
    maps = make_in_maps(inputs, L, list(range(8)))
    res = run_bass_kernel_spmd(nc, maps, core_ids=list(range(8)))
    return np.stack([r["out"] for r in res.results], axis=0).astype(np.float32)
```
